# Optimizing a Trainium2 kernel written in Bass

```python
import math
import jax, jax.numpy as jnp
from jax import lax
import numpy as np


D_MODEL = 1024
BATCH = 16
SEQ = 2048
DEPTH = 1

HEAD_DIM = 64
N_HEADS_MOBA = 8
N_HEADS_SB = 8
MOBA_WIDTH = N_HEADS_MOBA * HEAD_DIM
SB_WIDTH = N_HEADS_SB * HEAD_DIM
N_BRANCHES = 2
IN_WIDTH = 3 * MOBA_WIDTH + 3 * SB_WIDTH + N_BRANCHES * D_MODEL
MOBA_BLOCK = 256
MOBA_TOPK = 3
MOBA_Q_CHUNK = 16
SB_Q_BLOCK = 128
D_FF = 2816
CONV_WIDTH = 3
REL_BUCKETS = 32
REL_MAX_DIST = 128
NORM_EPS = 1e-6
NEG_INF = -1e30

kernel_name = 'hybrid_moba_stickbreaking_convffn_block'


def rms_norm(x, g):
    xf = x.astype(jnp.float32)
    y = xf * lax.rsqrt(jnp.mean(xf * xf, axis=-1, keepdims=True) + NORM_EPS)
    return (y * g.astype(jnp.float32)).astype(x.dtype)


def modulate(h, shift, scale):
    return h * (1.0 + scale[:, None, :]) + shift[:, None, :]


def to_heads(t, n_heads):
    b, s, _ = t.shape
    return t.reshape(b, s, n_heads, HEAD_DIM).transpose(0, 2, 1, 3)


def merge_heads(t):
    b, h, s, hd = t.shape
    return t.transpose(0, 2, 1, 3).reshape(b, s, h * hd)


def t5_bucket(rel):
    n = jnp.maximum(rel, 0)
    max_exact = REL_BUCKETS // 2
    n_f = jnp.maximum(n, max_exact).astype(jnp.float32)
    large = max_exact + (jnp.log(n_f / max_exact) / math.log(REL_MAX_DIST / max_exact)
                         * (REL_BUCKETS - max_exact)).astype(jnp.int32)
    large = jnp.minimum(large, REL_BUCKETS - 1)
    return jnp.where(n < max_exact, n, large)


def moba_attention(q, k, v, rel_table):
    b, h, s, hd = q.shape
    n_blk = -(-s // MOBA_BLOCK)
    s_pad = n_blk * MOBA_BLOCK
    pad = ((0, 0), (0, 0), (0, s_pad - s), (0, 0))
    q = jnp.pad(q, pad)
    k = jnp.pad(k, pad)
    v = jnp.pad(v, pad)
    k_blk = k.reshape(b, h, n_blk, MOBA_BLOCK, hd)
    v_blk = v.reshape(b, h, n_blk, MOBA_BLOCK, hd)
    k_mean = jnp.mean(k_blk.astype(jnp.float32), axis=3).astype(q.dtype)
    n_sel = min(MOBA_TOPK, n_blk)
    scale = hd ** -0.5
    n_chunks = s_pad // MOBA_Q_CHUNK
    q_chunks = jnp.moveaxis(q.reshape(b, h, n_chunks, MOBA_Q_CHUNK, hd), 2, 0)
    b_idx = jnp.arange(b)[:, None, None, None]
    h_idx = jnp.arange(h)[None, :, None, None]
    offs = jnp.arange(MOBA_BLOCK)
    blk_ids = jnp.arange(n_blk)

    def chunk(args):
        qc, ci = args
        q_start = ci * MOBA_Q_CHUNK
        q_pos = q_start + jnp.arange(MOBA_Q_CHUNK)
        own = q_start // MOBA_BLOCK
        gate = jnp.einsum('bhqd,bhnd->bhqn', qc, k_mean).astype(jnp.float32)
        gate = jnp.where(blk_ids < own, gate, NEG_INF)
        _, sel = lax.top_k(gate, n_sel)
        sel_ok = sel < own
        k_sel = k_blk[b_idx, h_idx, sel]
        v_sel = v_blk[b_idx, h_idx, sel]
        s_sel = jnp.einsum('bhqd,bhqnkd->bhqnk', qc, k_sel).astype(jnp.float32) * scale
        rel_sel = q_pos[:, None, None] - (sel[..., None] * MOBA_BLOCK + offs)
        bias_sel = rel_table[h_idx[..., None], t5_bucket(rel_sel)]
        s_sel = jnp.where(sel_ok[..., None], s_sel + bias_sel, NEG_INF)
        k_own = lax.dynamic_slice_in_dim(k, own * MOBA_BLOCK, MOBA_BLOCK, axis=2)
        v_own = lax.dynamic_slice_in_dim(v, own * MOBA_BLOCK, MOBA_BLOCK, axis=2)
        s_own = jnp.einsum('bhqd,bhkd->bhqk', qc, k_own).astype(jnp.float32) * scale
        rel_own = q_pos[:, None] - (own * MOBA_BLOCK + offs)[None, :]
        bias_own = rel_table[:, t5_bucket(rel_own)]
        s_own = jnp.where(rel_own >= 0, s_own + bias_own, NEG_INF)
        logits = jnp.concatenate(
            [s_sel.reshape(b, h, MOBA_Q_CHUNK, n_sel * MOBA_BLOCK), s_own], axis=-1)
        p = jax.nn.softmax(logits, axis=-1).astype(v.dtype)
        p_sel = p[..., :n_sel * MOBA_BLOCK].reshape(b, h, MOBA_Q_CHUNK, n_sel, MOBA_BLOCK)
        p_own = p[..., n_sel * MOBA_BLOCK:]
        return (jnp.einsum('bhqnk,bhqnkd->bhqd', p_sel, v_sel)
                + jnp.einsum('bhqk,bhkd->bhqd', p_own, v_own))

    out = lax.map(chunk, (q_chunks, jnp.arange(n_chunks)))
    return jnp.moveaxis(out, 0, 2).reshape(b, h, s_pad, hd)[:, :, :s]


def stick_breaking_attention(q, k, v):
    b, h, s, hd = q.shape
    n_qb = s // SB_Q_BLOCK
    scale = hd ** -0.5
    k_pos = jnp.arange(s)
    q_blocks = jnp.moveaxis(q.reshape(b, h, n_qb, SB_Q_BLOCK, hd), 2, 0)

    def block(args):
        qb, bi = args
        q_pos = bi * SB_Q_BLOCK + jnp.arange(SB_Q_BLOCK)
        z = jnp.einsum('bhqd,bhkd->bhqk', qb, k).astype(jnp.float32) * scale
        past = k_pos[None, :] < q_pos[:, None]
        log_beta = jax.nn.log_sigmoid(z)
        log_keep = jnp.where(past, jax.nn.log_sigmoid(-z), 0.0)
        log_later = lax.cumsum(log_keep, axis=3, reverse=True) - log_keep
        a = jnp.where(past, jnp.exp(log_beta + log_later), 0.0)
        return jnp.einsum('bhqk,bhkd->bhqd', a.astype(v.dtype), v)

    out = lax.map(block, (q_blocks, jnp.arange(n_qb)))
    return jnp.moveaxis(out, 0, 2).reshape(b, h, s, hd)


def causal_depthwise_conv(u, w, bias):
    ch = u.shape[-1]
    out = lax.conv_general_dilated(
        u, w[:, None, :], window_strides=(1,), padding=[(CONV_WIDTH - 1, 0)],
        dimension_numbers=('NWC', 'WIO', 'NWC'), feature_group_count=ch)
    return out + bias


def setup_inputs(seed: int = 0) -> dict:
    key = jax.random.key(seed)
    ks = jax.random.split(key, 16)
    f32 = jnp.float32
    nrm = lambda k, shp, sc: jax.random.normal(k, shp, f32) * sc
    return {
        'x': nrm(ks[0], (BATCH, SEQ, D_MODEL), 1.0),
        'c': nrm(ks[1], (BATCH, D_MODEL), 1.0),
        'w_ada': nrm(ks[2], (DEPTH, D_MODEL, 6 * D_MODEL), D_MODEL ** -0.5),
        'b_ada': nrm(ks[3], (DEPTH, 6 * D_MODEL), 0.02),
        'g_mix': 1.0 + nrm(ks[4], (DEPTH, D_MODEL), 0.05),
        'w_in': nrm(ks[5], (DEPTH, D_MODEL, IN_WIDTH), D_MODEL ** -0.5),
        'w_br_moba': nrm(ks[6], (DEPTH, MOBA_WIDTH, D_MODEL), MOBA_WIDTH ** -0.5),
        'w_br_sb': nrm(ks[7], (DEPTH, SB_WIDTH, D_MODEL), SB_WIDTH ** -0.5),
        'w_out': nrm(ks[8], (DEPTH, D_MODEL, D_MODEL), D_MODEL ** -0.5),
        'rel_bias': nrm(ks[9], (N_HEADS_MOBA, REL_BUCKETS), 0.5),
        'g_ffn': 1.0 + nrm(ks[10], (DEPTH, D_MODEL), 0.05),
        'w_up': nrm(ks[11], (DEPTH, D_MODEL, 2 * D_FF), D_MODEL ** -0.5),
        'w_conv': nrm(ks[12], (DEPTH, CONV_WIDTH, 2 * D_FF), CONV_WIDTH ** -0.5),
        'b_conv': nrm(ks[13], (DEPTH, 2 * D_FF), 0.02),
        'w_down': nrm(ks[14], (DEPTH, D_FF, D_MODEL), D_FF ** -0.5),
        'g_final': 1.0 + nrm(ks[15], (D_MODEL,), 0.05),
    }


def reference(x, c, w_ada, b_ada, g_mix, w_in, w_br_moba, w_br_sb, w_out, rel_bias,
              g_ffn, w_up, w_conv, b_conv, w_down, g_final):
    splits = list(np.cumsum([MOBA_WIDTH, MOBA_WIDTH, MOBA_WIDTH, SB_WIDTH, SB_WIDTH, SB_WIDTH]))
    c_act = jax.nn.silu(c)
    for l in range(DEPTH):
        mod = c_act @ w_ada[l] + b_ada[l]
        sh_m, sc_m, gt_m, sh_f, sc_f, gt_f = jnp.split(mod, 6, axis=-1)
        h = modulate(rms_norm(x, g_mix[l]), sh_m, sc_m)
        proj = jnp.einsum('bsd,de->bse', h, w_in[l])
        qa, ka, va, qb, kb, vb, gate_logits = jnp.split(proj, splits, axis=-1)
        oa = moba_attention(to_heads(qa, N_HEADS_MOBA), to_heads(ka, N_HEADS_MOBA),
                            to_heads(va, N_HEADS_MOBA), rel_bias)
        ob = stick_breaking_attention(to_heads(qb, N_HEADS_SB), to_heads(kb, N_HEADS_SB),
                                      to_heads(vb, N_HEADS_SB))
        ya = jnp.einsum('bse,ed->bsd', merge_heads(oa), w_br_moba[l])
        yb = jnp.einsum('bse,ed->bsd', merge_heads(ob), w_br_sb[l])
        gates = jax.nn.sigmoid(gate_logits.astype(jnp.float32)).astype(x.dtype)
        g_a, g_b = jnp.split(gates, 2, axis=-1)
        mixed = jnp.einsum('bsd,de->bse', g_a * ya + g_b * yb, w_out[l])
        x = x + gt_m[:, None, :] * mixed
        h2 = modulate(rms_norm(x, g_ffn[l]), sh_f, sc_f)
        u = jnp.einsum('bsd,df->bsf', h2, w_up[l])
        u = causal_depthwise_conv(u, w_conv[l], b_conv[l])
        u_val, u_gate = jnp.split(u, 2, axis=-1)
        y = jnp.einsum('bsf,fd->bsd', jax.nn.gelu(u_gate) * u_val, w_down[l])
        x = x + gt_f[:, None, :] * y
    return rms_norm(x, g_final)
```

```python
import contextlib
import numpy as np
import concourse.bass as bass
import concourse.mybir as mybir
from concourse.bass_utils import run_bass_kernel_spmd

F32 = mybir.dt.float32
BF16 = mybir.dt.bfloat16
AF = mybir.ActivationFunctionType
ALU = mybir.AluOpType
AX = mybir.AxisListType

NCORES = 8
SEQ = 2048
D = 1024
NSEQ = 2
DFF = 2816
NEG = -30000.0

DEBUG = False

NDMASEM = 20


class Buf:
    __slots__ = ("name", "w", "r", "rd")

    def __init__(self, name):
        self.name = name
        self.w = None
        self.r = {}
        self.rd = []


class Op:
    __slots__ = ("eng", "fn", "deps", "signal", "semval", "dma", "dsem", "prewait")

    def __init__(self, eng, fn, dma):
        self.eng = eng
        self.fn = fn
        self.deps = []
        self.signal = False
        self.semval = 0
        self.dma = dma
        self.dsem = None
        self.prewait = None


class Sched:
    ENGS = ("pe", "act", "dve", "pool", "sp")

    def __init__(self, nc):
        self.nc = nc
        self.ops = {e: [] for e in self.ENGS}
        self.ndma = {e: 0 for e in self.ENGS}
        self.bufs = {}

    def _B(self, x):
        b = self.bufs.get(x)
        if b is None:
            b = Buf(x)
            self.bufs[x] = b
        return b

    def op(self, eng, fn, reads=(), writes=(), dma=False):
        o = Op(eng, fn, dma)
        deps = []
        for b in reads:
            b = self._B(b)
            if b.w is not None:
                deps.append(b.w)
            if b.name.startswith("ps"):
                for e2, r2 in b.r.items():
                    if e2 != eng:
                        deps.append(r2)
        for b in writes:
            b = self._B(b)
            if b.w is not None:
                deps.append(b.w)
            deps.extend(b.r.values())
            deps.extend(b.rd)
        for b in writes:
            b = self._B(b)
            b.w = o
            b.r = {}
            b.rd = []
        for b in reads:
            b = self._B(b)
            if b.w is not o:
                if dma:
                    b.rd.append(o)
                else:
                    b.r[eng] = o
        seen = set()
        for d in deps:
            if d is o or id(d) in seen:
                continue
            if d.eng == "pe" and eng == "pe" and not d.dma and not dma:
                continue
            seen.add(id(d))
            o.deps.append(d)
            d.signal = True
        if dma:
            k = self.ndma[eng]
            self.ndma[eng] = k + 1
            o.dsem = (eng, k % NDMASEM)
            o.semval = 16 * (k // NDMASEM + 1)
            if k >= NDMASEM:
                o.prewait = (o.dsem, 16 * (k // NDMASEM))
        self.ops[eng].append(o)
        return o

    def barrier(self):
        lasts = []
        for e in self.ENGS:
            for o in reversed(self.ops[e]):
                if not o.dma and o.fn is not None:
                    lasts.append(o)
                    break
            seen = set()
            for o in reversed(self.ops[e]):
                if o.dma and o.dsem not in seen:
                    seen.add(o.dsem)
                    lasts.append(o)
                    if len(seen) >= NDMASEM:
                        break
        for e in self.ENGS:
            o = Op(e, None, False)
            for d in lasts:
                if d.eng == e and not d.dma:
                    continue
                o.deps.append(d)
                d.signal = True
            self.ops[e].append(o)
        for b in self.bufs.values():
            b.w = None
            b.r = {}
            b.rd = []

    def emit(self):
        nc = self.nc
        for e in self.ENGS:
            c = 0
            for o in self.ops[e]:
                if o.dma:
                    continue
                if o.signal:
                    c += 1
                    o.semval = c
        with contextlib.ExitStack() as st:
            esem = {e: st.enter_context(nc.semaphore("s_" + e)) for e in self.ENGS}
            dsem = {}
            for e in self.ENGS:
                for i in range(min(NDMASEM, self.ndma[e])):
                    dsem[(e, i)] = st.enter_context(nc.semaphore("d_%s_%d" % (e, i)))
            block = st.enter_context(nc.Block())

            def run(ename, eng):
                waited = {}

                def wait(key, sem, val):
                    if waited.get(key, 0) >= val:
                        return
                    waited[key] = val
                    eng.wait_ge(sem, val)

                for o in self.ops[ename]:
                    if o.prewait is not None:
                        ds, v = o.prewait
                        wait(ds, dsem[ds], v)
                    for d in o.deps:
                        if d.dma:
                            wait(d.dsem, dsem[d.dsem], d.semval)
                        else:
                            wait(d.eng, esem[d.eng], d.semval)
                    if o.fn is None:
                        continue
                    ins = o.fn(eng)
                    if o.dma:
                        ins.then_inc(dsem[o.dsem], 16)
                    elif o.signal:
                        ins.then_inc(esem[ename], 1)

            @block.tensor
            def _(pe):
                run("pe", pe)

            @block.scalar
            def _(act):
                run("act", act)

            @block.vector
            def _(dve):
                run("dve", dve)

            @block.gpsimd
            def _(pool):
                run("pool", pool)

            @block.sync
            def _(sp):
                run("sp", sp)


def _bucket_table(nmax):
    n = np.arange(nmax, dtype=np.float64)
    nf = np.maximum(n, 16.0)
    large = 16 + np.floor(np.log(nf / 16.0) / np.log(8.0) * 16.0 + 1e-9).astype(np.int64)
    large = np.minimum(large, 31)
    return np.where(n < 16, n.astype(np.int64), large)


def _consts():
    i = np.arange(128)[:, None]
    c = np.arange(1024)[None, :]
    rel = c - 384 - i
    bt = _bucket_table(1024)
    bidx = bt[np.maximum(rel, 0)]
    mmask = np.where(rel < 0, 8.0 * NEG, 0.0).astype(np.float32)
    mpast = (rel > 0).astype(np.float32)
    negm = np.where(rel > 0, 0.0, NEG).astype(np.float32)
    ident = np.eye(128, dtype=np.float32)
    ustrict = (np.arange(128)[:, None] > np.arange(128)[None, :]).astype(np.float32)
    sel = np.zeros((16, 16, 128), np.float32)
    for w in range(16):
        sel[w, w, :] = 1.0
    sel = sel.reshape(16, 16 * 128)
    inv = np.zeros((16, 2, 8), np.float32)
    for t in range(16):
        inv[t, :, (t // 2):] = 1.0
    inv = np.broadcast_to(inv.reshape(1, 256), (128, 256)).copy()
    selrow = np.zeros((2, 2, 128), np.float32)
    selrow[0, 0, :] = 1.0
    selrow[1, 1, :] = 1.0
    selrow = selrow.reshape(2, 256)
    return dict(bidx=bidx, mmask=mmask, mpast=mpast, negm=negm, ident=ident, ustrict=ustrict,
                sel=sel, inv=inv, selrow=selrow)


def build(stop_after=None):
    nc = bass.Bass("TRN2", target_bir_lowering=False)

    def din(name, shape, dt=F32):
        return nc.dram_tensor(name, list(shape), dt, kind="ExternalInput").ap()

    x = din("x", [NSEQ, SEQ, D])
    cT = din("cT", [128, 8, 2])
    w_ada = din("w_ada", [D, 6 * D])
    b_adaT = din("b_adaT", [128, 48])
    b_ada_row = din("b_ada_row", [1, 2048])
    g_mixT = din("g_mixT", [128, 8])
    g_ffnT = din("g_ffnT", [128, 8])
    g_fin = din("g_fin", [1, D])
    w_in = din("w_in", [D, 5120])
    w_bra = din("w_bra", [512, D])
    w_brb = din("w_brb", [512, D])
    w_out = din("w_out", [D, D])
    w_up = din("w_up", [D, 2 * DFF])
    w_convT = din("w_convT", [128, 44, 3])
    b_convT = din("b_convT", [128, 44])
    w_down = din("w_down", [DFF, D])
    relG = din("relG", [8, 128, 1024])
    b31 = din("b31", [128, 8])
    k_mmask = din("k_mmask", [128, 1024])
    k_mpast = din("k_mpast", [128, 1024])
    k_negm = din("k_negm", [128, 1024])
    k_ident = din("k_ident", [128, 128])
    k_ustrict = din("k_ustrict", [128, 128])
    k_sel = din("k_sel", [16, 2048])
    k_inv = din("k_inv", [128, 256])
    k_selrow = din("k_selrow", [2, 256])
    out = nc.dram_tensor("out", [NSEQ, SEQ, D], F32, kind="ExternalOutput").ap()
    x1s = nc.dram_tensor("x1s", [NSEQ, SEQ, D], F32, kind="Internal").ap()
    dbg = {}
    if DEBUG:
        dbg["hT"] = nc.dram_tensor("dbg_hT", [128, 8 * SEQ], BF16, kind="ExternalOutput").ap()
        dbg["oT"] = nc.dram_tensor("dbg_oT", [128, 8 * SEQ], BF16, kind="ExternalOutput").ap()
        dbg["combT"] = nc.dram_tensor("dbg_combT", [128, 8 * SEQ], BF16, kind="ExternalOutput").ap()
        dbg["h2T"] = nc.dram_tensor("dbg_h2T", [128, 8 * SEQ], BF16, kind="ExternalOutput").ap()
        dbg["yT"] = nc.dram_tensor("dbg_yT", [128, 22 * SEQ], BF16, kind="ExternalOutput").ap()
        dbg["misc"] = nc.dram_tensor("dbg_misc", [128, 1024], F32, kind="ExternalOutput").ap()

    K = 1024
    TOTAL = 207 * K
    with contextlib.ExitStack() as st:
        big = st.enter_context(nc.sbuf_tensor("big", [128, TOTAL // 4], F32))
        PS = [st.enter_context(nc.psum_tensor("ps%d" % i, [128, 512], F32)) for i in range(8)]
        S = Sched(nc)

        def VW(off, nbytes, dt=F32, parts=128):
            assert off % 4 == 0 and nbytes % 4 == 0 and off + nbytes <= TOTAL, (off, nbytes)
            v = big[0:parts, off // 4:(off + nbytes) // 4]
            if dt == BF16:
                v = v.bitcast(BF16)
            return v

        def psb(i):
            return PS[i][:, :].bitcast(BF16)

        A_OFF = 0
        B_OFF = 32 * K
        C_OFF = 64 * K
        Y_OFF = 32 * K
        D_OFF = 120 * K
        D_SZ = 71 * K
        CO = 191 * K

        hT = VW(A_OFF, 32 * K, BF16).rearrange("p (k n) -> p k n", k=8)
        oT = VW(B_OFF, 32 * K, BF16).rearrange("p (k n) -> p k n", k=8)
        combT = VW(C_OFF, 32 * K, BF16).rearrange("p (k n) -> p k n", k=8)
        Bh = VW(C_OFF, 16 * K, BF16).rearrange("p (h n) -> p h n", h=8)
        yT = VW(Y_OFF, 88 * K, BF16).rearrange("p (k n) -> p k n", k=22)

        co = [CO]

        def calloc(nbytes, dt=F32, parts=128):
            v = VW(co[0], nbytes, dt, parts)
            co[0] += (nbytes + 31) // 32 * 32
            assert co[0] <= TOTAL
            return v

        identb = calloc(256, BF16)
        ustrict = calloc(256, BF16)
        onesb = calloc(256, BF16)
        onesf = calloc(512, F32)
        selb = calloc(4096, BF16)
        selrow = calloc(1024, F32)
        mpast = calloc(2048, BF16)
        negm = calloc(2048, BF16)
        inv = calloc(1024, F32)
        neginv = calloc(1024, F32)
        b31s = calloc(32, F32)
        modT = calloc(384, F32).rearrange("p (c b) -> p c b", b=2)
        A1 = calloc(64, F32).rearrange("p (c b) -> p c b", b=2)
        A2 = calloc(64, F32).rearrange("p (c b) -> p c b", b=2)
        gmixT = calloc(32, F32)
        gffnT = calloc(32, F32)
        wconv = calloc(44 * 3 * 4, F32).rearrange("p (c i) -> p c i", i=3)
        bconv = calloc(44 * 4, F32)
        badaT = calloc(48 * 4, F32)
        small = calloc(256, F32)
        CONST_END = co[0]

        def dma(eng, out_, in_, reads=(), writes=()):
            return S.op(eng, lambda e: e.dma_start(out=out_, in_=in_), reads=reads, writes=writes, dma=True)

        def mm(o, l, r, start, stop, reads, writes):
            return S.op("pe", lambda e: e.matmul(o, lhsT=l, rhs=r, start=start, stop=stop), reads=reads, writes=writes)

        def act(o, i, func, reads, writes, bias=None, scale=None, accum=None):
            kw = {}
            if bias is not None:
                kw["bias"] = bias
            if scale is not None:
                kw["scale"] = scale
            if accum is not None:
                kw["accum_out"] = accum
            return S.op("act", lambda e: e.activation(out=o, in_=i, func=func, **kw), reads=reads, writes=writes)

        def tsc(eng, o, i, s1, s2, op0, op1, reads, writes):
            if s2 is None:
                return S.op(eng, lambda e: e.tensor_scalar(out=o, in0=i, scalar1=s1, scalar2=None, op0=op0), reads=reads, writes=writes)
            return S.op(eng, lambda e: e.tensor_scalar(out=o, in0=i, scalar1=s1, scalar2=s2, op0=op0, op1=op1), reads=reads, writes=writes)

        def tt(eng, o, a, b, op, reads, writes):
            return S.op(eng, lambda e: e.tensor_tensor(out=o, in0=a, in1=b, op=op), reads=reads, writes=writes)

        def stt(o, a, s, b, op0, op1, reads, writes, accum=None):
            if accum is None:
                return S.op("dve", lambda e: e.scalar_tensor_tensor(out=o, in0=a, scalar=s, in1=b, op0=op0, op1=op1), reads=reads, writes=writes)
            return S.op("dve", lambda e: e.scalar_tensor_tensor(out=o, in0=a, scalar=s, in1=b, op0=op0, op1=op1, accum_out=accum), reads=reads, writes=writes)

        def cp(eng, o, i, reads, writes):
            if eng == "act":
                return S.op("act", lambda e: e.activation(out=o, in_=i, func=AF.Identity), reads=reads, writes=writes)
            return S.op(eng, lambda e: e.tensor_copy(out=o, in_=i), reads=reads, writes=writes)

        stg = VW(D_OFF, 4096, F32)
        stg2 = VW(D_OFF + 4096, 4096, F32)
        dma("sp", stg[:, 0:128], k_ident, writes=["stg"])
        cp("dve", identb, stg[:, 0:128], ["stg"], ["identb"])
        dma("sp", stg[:, 128:256], k_ustrict, writes=["stgb"])
        cp("dve", ustrict, stg[:, 128:256], ["stgb"], ["ustrict"])
        S.op("pool", lambda e: e.memset(onesb, 1.0), writes=["onesb"])
        S.op("pool", lambda e: e.memset(onesf, 1.0), writes=["onesf"])
        dma("pool", selb[0:16, :], k_sel, writes=["selb"])
        dma("sp", selrow[0:2, :], k_selrow, writes=["selrow"])
        dma("pool", mpast, k_mpast, writes=["mpast"])
        dma("pool", negm, k_negm, writes=["negm"])
        dma("sp", inv, k_inv, writes=["inv"])
        tsc("dve", neginv, inv, -1e30, None, ALU.mult, None, ["inv"], ["neginv"])
        dma("sp", b31s, b31, writes=["b31s"])
        dma("sp", gmixT, g_mixT, writes=["gmixT"])
        dma("sp", gffnT, g_ffnT, writes=["gffnT"])
        dma("sp", wconv, w_convT, writes=["wconv"])
        dma("sp", bconv, b_convT, writes=["bconv"])
        dma("sp", badaT, b_adaT, writes=["badaT"])

        cTt = VW(D_OFF + 8192, 64, F32).rearrange("p (k b) -> p k b", b=2)
        cactb = VW(D_OFF + 8192 + 64, 32, BF16).rearrange("p (k b) -> p k b", b=2)
        cactf = VW(D_OFF + 8192 + 128, 64, F32).rearrange("p (k b) -> p k b", b=2)
        dma("sp", cTt, cT, writes=["cTt"])
        act(cactb, cTt, AF.Silu, ["cTt"], ["cactb"])
        act(cactf, cTt, AF.Silu, ["cTt"], ["cactf"])
        gtrow = VW(D_OFF + 8192 + 256, 8192, F32, parts=2)
        badar = VW(D_OFF + 8192 + 256 + 8192, 8192, F32, parts=2)
        dma("sp", badar[0:1, :], b_ada_row, writes=["badar0"])
        dma("sp", badar[1:2, :], b_ada_row, writes=["badar1"])
        WB_OFF = D_OFF + 32 * K
        wada_v = w_ada.rearrange("(k p) n -> p k n", p=128)
        for blk in range(8):
            wb = VW(WB_OFF + (blk % 2) * 12 * K, 12 * K, BF16).rearrange("p (k n) -> p k n", k=8)
            wn = "wada%d" % (blk % 2)
            dma("pool", wb, wada_v[:, :, blk * 768:(blk + 1) * 768], writes=[wn])
            for j in range(6):
                cidx = blk * 6 + j
                for k in range(8):
                    mm(PS[0][:, 2 * cidx:2 * cidx + 2], wb[:, k, j * 128:(j + 1) * 128], cactb[:, k, :], k == 0, k == 7,
                       [wn, "cactb"], ["ps0"])
        tt("dve", modT, PS[0][:, 0:96].rearrange("p (c b) -> p c b", b=2),
           badaT.rearrange("p (c o) -> p c o", o=1).to_broadcast([128, 48, 2]), ALU.add, ["ps0", "badaT"], ["modT"])
        tsc("dve", A1, modT[:, 8:16, :], 1.0, None, ALU.add, None, ["modT"], ["A1"])
        tt("dve", A1, A1, gmixT.rearrange("p (c o) -> p c o", o=1).to_broadcast([128, 8, 2]), ALU.mult, ["A1", "gmixT"], ["A1"])
        tsc("dve", A2, modT[:, 32:40, :], 1.0, None, ALU.add, None, ["modT"], ["A2"])
        tt("dve", A2, A2, gffnT.rearrange("p (c o) -> p c o", o=1).to_broadcast([128, 8, 2]), ALU.mult, ["A2", "gffnT"], ["A2"])
        GT_OFF = D_OFF + 32 * K + 24 * K
        S.barrier()
        wf = VW(D_OFF + 26 * K, 32 * K, F32).rearrange("p (k n) -> p k n", k=8)
        for g, c0 in enumerate((2048, 5120)):
            dma("sp", wf, wada_v[:, :, c0:c0 + 1024], writes=["wf"])
            for hh in range(2):
                for k in range(8):
                    mm(PS[1][0:2, :], cactf[:, k, :], wf[:, k, hh * 512:(hh + 1) * 512], k == 0, k == 7, ["wf", "cactf"], ["ps1"])
                tt("dve", gtrow[0:2, g * 1024 + hh * 512:g * 1024 + (hh + 1) * 512], PS[1][0:2, :],
                   badar[0:2, g * 1024 + hh * 512:g * 1024 + (hh + 1) * 512], ALU.add, ["ps1", "badar0", "badar1"], ["gtrow"])
        GTB = D_OFF + D_SZ - 16 * K

        def GT(s, g):
            return VW(GTB + (s * 2 + g) * 4096, 4096, F32)

        for s in range(NSEQ):
            for g in range(2):
                for hh in range(2):
                    mm(PS[2][:, :], selrow[0:2, s * 128:(s + 1) * 128], gtrow[0:2, g * 1024 + hh * 512:g * 1024 + (hh + 1) * 512],
                       True, True, ["selrow", "gtrow"], ["ps2"])
                    cp("act", GT(s, g)[:, hh * 512:(hh + 1) * 512], PS[2][:, :], ["ps2"], ["GT"])
        S.barrier()
        STOP = [False]

        def stop(tag):
            if stop_after == tag:
                STOP[0] = True
            return STOP[0]
        DW = GTB - D_OFF

        def rms_tile(xt_ap, xtn, ssn, tag):
            junk = VW(D_OFF + 0, 4096, F32)
            ss = small[:, 0:1]
            lnv = small[:, 1:2]
            rstd = small[:, 2:3]
            stt(junk, xt_ap, 1.0, xt_ap, ALU.mult, ALU.mult, [xtn], ["junk", "ss"], accum=ss)
            act(lnv, ss, AF.Ln, ["ss"], ["lnv"], bias=1e-6, scale=1.0 / 1024.0)
            act(rstd, lnv, AF.Exp, ["lnv"], ["rstd"], scale=-0.5)
            return rstd

        def norm_to_T(xt_ap, xtn, dstT, Amod, Bmod_c0, s, t, xnb, xnbn, psi):
            rstd = rms_tile(xt_ap, xtn, None, None)
            tsc("dve", xnb, xt_ap, rstd, None, ALU.mult, None, [xtn, "rstd"], [xnbn])
            pn = "ps%d" % psi
            pv = psb(psi)
            for c in range(8):
                S.op("pe", lambda e, c=c: e.transpose(out=pv[:, c * 128:(c + 1) * 128], in_=xnb[:, c * 128:(c + 1) * 128], identity=identb),
                     reads=[xnbn, "identb"], writes=[pn])
            for c in range(8):
                o = dstT[:, c, t * 128:(t + 1) * 128]
                i = pv[:, c * 128:(c + 1) * 128]
                if t % 2 == 0:
                    act(o, i, AF.Identity, [pn, "A1", "A2", "modT"], ["dstT"], bias=modT[:, Bmod_c0 + c, s:s + 1], scale=Amod[:, c, s:s + 1])
                else:
                    tsc("dve", o, i, Amod[:, c, s:s + 1], modT[:, Bmod_c0 + c, s:s + 1], ALU.mult, ALU.add, [pn, "A1", "A2", "modT"], ["dstT"])

        w_in_v = w_in.rearrange("(k p) n -> p k n", p=128)

        for s in range(NSEQ):
            if stop("setup"):
                break
            for t in range(16):
                xt = VW(D_OFF + 4096 + (t % 2) * 4096, 4096, F32)
                xtn = "xt%d" % (t % 2)
                dma("sp", xt, x[s, t * 128:(t + 1) * 128, :], writes=[xtn])
                xnb = VW(D_OFF + 12288 + (t % 2) * 2048, 2048, BF16)
                norm_to_T(xt, xtn, hT, A1, 0, s, t, xnb, "xnb%d" % (t % 2), 6 + (t % 2))
            S.barrier()
            if DEBUG and s == 0:
                dma("sp", dbg["hT"], VW(A_OFF, 32 * K, BF16), reads=["dstT"])
                S.barrier()

            if stop("p1"):
                break
            mm_f = VW(D_OFF, 4096, F32)
            dma("sp", mm_f, k_mmask, writes=["mm_f"])
            for h in range(8):
                gtile = VW(D_OFF + 4096 + (h % 2) * 4096, 4096, F32)
                gn = "gt%d" % (h % 2)
                dma("sp", gtile, relG[h], writes=[gn])
                tsc("dve", gtile, gtile, b31s[:, h:h + 1], 8.0, ALU.subtract, ALU.mult, [gn, "b31s"], [gn])
                tt("dve", Bh[:, h, :], gtile, mm_f, ALU.add, [gn, "mm_f"], ["Bh"])
            S.barrier()
            o_ = [D_OFF]

            def dalloc(nbytes, dt=F32, parts=128):
                v = VW(o_[0], nbytes, dt, parts)
                o_[0] += (nbytes + 31) // 32 * 32
                assert o_[0] <= GTB, o_[0] - GTB
                return v

            wq = [dalloc(2048, BF16).rearrange("p (k n) -> p k n", k=8) for _ in range(1)]
            wk = [dalloc(2048, BF16).rearrange("p (k n) -> p k n", k=8) for _ in range(1)]
            wv = [dalloc(2048, BF16).rearrange("p (k n) -> p k n", k=8) for _ in range(1)]
            qT = dalloc(4096, BF16)
            kT = dalloc(4096, BF16)
            Vaug = dalloc(8192, BF16).rearrange("p (t n) -> p t n", t=16)
            negT = dalloc(4096, BF16)
            gm = dalloc(1024, F32)
            top8 = dalloc(1024, F32)
            mge = dalloc(1024, F32)
            negq = dalloc(512, BF16)
            kms = dalloc(32, F32)
            kmh = dalloc(16, BF16)
            kmr = dalloc(32, F32)
            kml = dalloc(16, BF16)
            PT = [dalloc(1024, BF16) for _ in range(3)]
            e_t = [dalloc(2048, F32) for _ in range(2)]
            sp_t = [dalloc(2048, F32) for _ in range(2)]
            lkm = [dalloc(1024, BF16) for _ in range(2)]
            ssum = [dalloc(1024, BF16) for _ in range(2)]
            t_t = [dalloc(2048, F32) for _ in range(2)]
            a_t = [dalloc(1024, BF16) for _ in range(2)]
            rr = e_t[0]
            lnr = e_t[1]
            bcs = sp_t[0]

            S.op("pool", lambda e: e.memset(Vaug, 0.0), writes=["Vaug"])
            S.op("pool", lambda e: e.memset(Vaug[:, :, 64:65], 1.0), reads=[], writes=["Vaug"])
            S.op("pool", lambda e: e.memset(Vaug[:, :, 128:129], 1.0), reads=[], writes=["Vaug"])

            zc = [0]

            def zbank():
                i = zc[0] % 3
                zc[0] += 1
                return i

            def project_pair(kind, p):
                base = 0 if kind == 0 else 1536
                pp = 0
                dma("pool", wq[pp], w_in_v[:, :, base + 128 * p:base + 128 * p + 128], writes=["wq%d" % pp])
                dma("pool", wk[pp], w_in_v[:, :, base + 512 + 128 * p:base + 512 + 128 * p + 128], writes=["wk%d" % pp])
                dma("pool", wv[pp], w_in_v[:, :, base + 1024 + 128 * p:base + 1024 + 128 * p + 128], writes=["wv%d" % pp])
                for (wt, wn, dst, dn) in ((wq[pp], "wq%d" % pp, qT, "qT"), (wk[pp], "wk%d" % pp, kT, "kT")):
                    for tq in range(4):
                        zi = zbank()
                        for k in range(8):
                            mm(PS[zi][:, :], wt[:, k, :], hT[:, k, tq * 512:(tq + 1) * 512], k == 0, k == 7, [wn, "hT"], ["ps%d" % zi])
                        cp("act" if tq % 2 == 0 else "dve", dst[:, tq * 512:(tq + 1) * 512], PS[zi][:, :], ["ps%d" % zi], [dn])
                for g in range(4):
                    zi = zbank()
                    for tl in range(4):
                        t = g * 4 + tl
                        for k in range(8):
                            mm(PS[zi][:, tl * 128:(tl + 1) * 128], hT[:, k, t * 128:(t + 1) * 128], wv[pp][:, k, :], k == 0, k == 7,
                               ["wv%d" % pp, "hT"], ["ps%d" % zi])
                    pv3 = PS[zi][:, :].rearrange("p (t n) -> p t n", t=4)
                    cp("act", Vaug[:, g * 4:(g + 1) * 4, 0:64], pv3[:, :, 0:64], ["ps%d" % zi], ["Vaug"])
                    cp("dve", Vaug[:, g * 4:(g + 1) * 4, 192:256], pv3[:, :, 64:128], ["ps%d" % zi], ["Vaug"])

            def moba_gate():
                tensor_reduce = lambda e: e.tensor_reduce(out=kms, in_=kT.rearrange("p (n j) -> p n j", j=256), axis=AX.X, op=ALU.add)
                S.op("dve", tensor_reduce, reads=["kT"], writes=["kms"])
                tsc("dve", kms, kms, 1.0 / 256.0, None, ALU.mult, None, ["kms"], ["kms"])
                cp("dve", kmh, kms, ["kms"], ["kmh"])
                tt("dve", kmr, kms, kmh, ALU.subtract, ["kms", "kmh"], ["kmr"])
                cp("dve", kml, kmr, ["kmr"], ["kml"])
                gv = PS[3][:, 0:256].rearrange("p (t h n) -> p t h n", t=16, h=2)
                for t in range(16):
                    for hd in range(2):
                        hs = slice(64 * hd, 64 * hd + 64)
                        mm(gv[:, t, hd, :], qT[hs, t * 128:(t + 1) * 128], kmh[hs, :], True, False, ["qT", "kmh"], ["ps3"])
                        mm(gv[:, t, hd, :], qT[hs, t * 128:(t + 1) * 128], kml[hs, :], False, True, ["qT", "kml"], ["ps3"])
                tt("dve", gm, PS[3][:, 0:256], neginv, ALU.add, ["ps3", "neginv"], ["gm"])
                gm3 = gm.rearrange("p (g n) -> p g n", n=8)
                t83 = top8.rearrange("p (g n) -> p g n", n=8)
                for g in range(32):
                    S.op("dve", lambda e, g=g: e.max(out=t83[:, g, :], in_=gm3[:, g, :]), reads=["gm"], writes=["top8"])
                tt("dve", mge.rearrange("p (g n) -> p g n", n=8), gm3, t83[:, :, 2:3].to_broadcast([128, 32, 8]), ALU.is_ge,
                   ["gm", "top8"], ["mge"])
                tt("dve", mge, mge, inv, ALU.max, ["mge", "inv"], ["mge"])
                tsc("dve", negq, mge, -8.0 * NEG, 8.0 * NEG, ALU.mult, ALU.add, ["mge"], ["negq"])
                for half in range(2):
                    pv = psb(4)
                    for tl in range(8):
                        t = half * 8 + tl
                        S.op("pe", lambda e, t=t, tl=tl, pv=pv: e.transpose(out=pv[0:16, tl * 128:(tl + 1) * 128], in_=negq[:, t * 16:(t + 1) * 16], identity=identb),
                             reads=["negq", "identb"], writes=["ps4"])
                    cp("dve", negT[0:16, half * 1024:(half + 1) * 1024], pv[0:16, :], ["ps4"], ["negT"])

            acc_c = [0]

            def moba_head(p, hd):
                head = 2 * p + hd
                hs = slice(64 * hd, 64 * hd + 64)
                for qt in range(4):
                    ai = 5 + (acc_c[0] % 2)
                    acc_c[0] += 1
                    an = "ps%d" % ai
                    nch = 4 * qt + 4
                    qs = slice(qt * 512, (qt + 1) * 512)
                    for kc in range(nch):
                        zi = zbank()
                        zn = "ps%d" % zi
                        n = kc // 2
                        d0 = 512 * qt - 128 * kc
                        use_sel = n <= 2 * qt
                        use_toe = d0 < 240
                        mm(PS[zi][:, :], kT[hs, kc * 128:(kc + 1) * 128], qT[hs, qs], True, not (use_sel or use_toe), ["kT", "qT"], [zn])
                        if use_sel:
                            w = hd * 8 + n
                            mm(PS[zi][:, :], selb[0:16, w * 128:(w + 1) * 128], negT[0:16, qs], False, not use_toe, ["selb", "negT"], [zn])
                        if use_toe:
                            c0 = d0 + 384
                            mm(PS[zi][:, :], identb, Bh[:, head, c0:c0 + 512], False, True, ["identb", "Bh"], [zn])
                        pi = kc % 3
                        act(PT[pi], PS[zi][:, :], AF.Exp, [zn, "b31s"], ["PT%d" % pi], bias=b31s[:, head:head + 1], scale=0.125)
                        if hd == 0:
                            mm(PS[ai][0:65, :], Vaug[:, kc, 0:65], PT[pi], kc == 0, kc == nch - 1, ["Vaug", "PT%d" % pi], [an])
                        else:
                            mm(PS[ai][:, :], Vaug[:, kc, 128:256], PT[pi], kc == 0, kc == nch - 1, ["Vaug", "PT%d" % pi], [an])
                    srow = 64 if hd == 0 else 0
                    act(lnr[0:1, 0:512], PS[ai][srow:srow + 1, :], AF.Ln, [an], ["e1"])
                    act(rr[0:1, 0:512], lnr[0:1, 0:512], AF.Exp, ["e1"], ["e0"], scale=-1.0)
                    mm(PS[7][:, :], onesf[0:1, :], rr[0:1, 0:512], True, True, ["onesf", "e0"], ["ps7"])
                    cp("act", bcs[hs, 0:512], PS[7][hs, :], ["ps7"], ["sp0"])
                    tt("dve", oT[hs, p, qs], PS[ai][hs, :], bcs[hs, 0:512], ALU.mult, [an, "sp0"], ["oT"])

            def sb_head(p, hd):
                hs = slice(64 * hd, 64 * hd + 64)
                for qt in range(4):
                    ai = 5 + (acc_c[0] % 2)
                    acc_c[0] += 1
                    an = "ps%d" % ai
                    nch = 4 * qt + 4
                    qs = slice(qt * 512, (qt + 1) * 512)
                    for idx in range(nch):
                        kc = nch - 1 - idx
                        b2 = idx % 2
                        zi = zbank()
                        zn = "ps%d" % zi
                        li = 3 + b2
                        ln_ = "ps%d" % li
                        d0 = 512 * qt - 128 * kc
                        diag = kc >= 4 * qt
                        c0 = d0 + 384
                        mm(PS[zi][:, :], kT[hs, kc * 128:(kc + 1) * 128], qT[hs, qs], True, True, ["kT", "qT"], [zn])
                        act(e_t[b2], PS[zi][:, :], AF.Exp, [zn], ["e%d" % b2], scale=-0.125)
                        act(sp_t[b2], e_t[b2], AF.Ln, ["e%d" % b2], ["sp%d" % b2], bias=1.0)
                        stt(lkm[b2], PS[zi][:, :], 0.125, sp_t[b2], ALU.mult, ALU.add, [zn, "sp%d" % b2], ["lkm%d" % b2])
                        if diag:
                            tt("pool", lkm[b2], lkm[b2], mpast[:, c0:c0 + 512], ALU.mult, ["lkm%d" % b2, "mpast"], ["lkm%d" % b2])
                        mm(PS[li][:, :], ustrict, lkm[b2], True, idx == 0, ["ustrict", "lkm%d" % b2], [ln_])
                        if idx > 0:
                            mm(PS[li][:, :], onesb, ssum[(idx - 1) % 2], False, True, ["onesb", "ssum%d" % ((idx - 1) % 2)], [ln_])
                        if kc > 0:
                            if idx == 0:
                                cp("pool", ssum[0], lkm[b2], ["lkm%d" % b2], ["ssum0"])
                            else:
                                tt("pool", ssum[idx % 2], ssum[(idx - 1) % 2], lkm[b2], ALU.add,
                                   ["ssum%d" % ((idx - 1) % 2), "lkm%d" % b2], ["ssum%d" % (idx % 2)])
                        stt(t_t[b2], PS[li][:, :], -1.0, sp_t[b2], ALU.mult, ALU.subtract, [ln_, "sp%d" % b2], ["t%d" % b2])
                        if diag:
                            tt("pool", t_t[b2], t_t[b2], negm[:, c0:c0 + 512], ALU.add, ["t%d" % b2, "negm"], ["t%d" % b2])
                        act(a_t[b2], t_t[b2], AF.Exp, ["t%d" % b2], ["a%d" % b2])
                        if hd == 0:
                            mm(PS[ai][0:64, :], Vaug[:, kc, 0:64], a_t[b2], idx == 0, kc == 0, ["Vaug", "a%d" % b2], [an])
                        else:
                            mm(PS[ai][:, :], Vaug[:, kc, 128:256], a_t[b2], idx == 0, kc == 0, ["Vaug", "a%d" % b2], [an])
                    cp("act", oT[hs, 4 + p, qs], PS[ai][hs, :], [an], ["oT"])

            for kind in range(2):
                for p in range(4):
                    project_pair(kind, p)
                    if kind == 0:
                        moba_gate()
                        for hd in range(2):
                            moba_head(p, hd)
                    else:
                        for hd in range(2):
                            sb_head(p, hd)
            S.barrier()
            if DEBUG and s == 0:
                dma("sp", dbg["oT"], VW(B_OFF, 32 * K, BF16), reads=["oT"])
                S.barrier()

            if stop("p2"):
                break
            o_[0] = D_OFF
            wbra = dalloc(8192, BF16).rearrange("p (k n) -> p k n", k=4)
            wbrb = dalloc(8192, BF16).rearrange("p (k n) -> p k n", k=4)
            wg = dalloc(16 * K, BF16).rearrange("p (k n) -> p k n", k=8)
            sg = [dalloc(2048, F32) for _ in range(2)]
            t1 = [dalloc(2048, F32) for _ in range(2)]
            t2 = [dalloc(2048, F32) for _ in range(2)]
            dma("pool", wbra, w_bra.rearrange("(k p) n -> p k n", p=128), writes=["wbra"])
            dma("pool", wbrb, w_brb.rearrange("(k p) n -> p k n", p=128), writes=["wbrb"])
            cnt = 0
            for cg in range(2):
                for j in range(4):
                    c = cg * 4 + j
                    dma("pool", wg[:, :, j * 256:j * 256 + 128], w_in_v[:, :, 3072 + 128 * c:3072 + 128 * c + 128], reads=[], writes=["wga%d" % j])
                    dma("pool", wg[:, :, j * 256 + 128:j * 256 + 256], w_in_v[:, :, 4096 + 128 * c:4096 + 128 * c + 128], reads=[], writes=["wgb%d" % j])
                for j in range(4):
                    c = cg * 4 + j
                    for tq in range(4):
                        ts_ = slice(tq * 512, (tq + 1) * 512)
                        b2 = cnt % 2
                        cnt += 1
                        pa, pb, pga, pgb = (0, 1, 2, 3) if b2 == 0 else (4, 5, 6, 7)
                        for k in range(4):
                            mm(PS[pa][:, :], wbra[:, k, c * 128:(c + 1) * 128], oT[:, k, ts_], k == 0, k == 3, ["wbra", "oT"], ["ps%d" % pa])
                        for k in range(4):
                            mm(PS[pb][:, :], wbrb[:, k, c * 128:(c + 1) * 128], oT[:, 4 + k, ts_], k == 0, k == 3, ["wbrb", "oT"], ["ps%d" % pb])
                        for k in range(8):
                            mm(PS[pga][:, :], wg[:, k, j * 256:j * 256 + 128], hT[:, k, ts_], k == 0, k == 7, ["wga%d" % j, "hT"], ["ps%d" % pga])
                        for k in range(8):
                            mm(PS[pgb][:, :], wg[:, k, j * 256 + 128:j * 256 + 256], hT[:, k, ts_], k == 0, k == 7, ["wgb%d" % j, "hT"], ["ps%d" % pgb])
                        act(sg[0], PS[pga][:, :], AF.Sigmoid, ["ps%d" % pga], ["sg0"])
                        act(sg[1], PS[pgb][:, :], AF.Sigmoid, ["ps%d" % pgb], ["sg1"])
                        tt("dve", t1[b2], PS[pa][:, :], sg[0], ALU.mult, ["ps%d" % pa, "sg0"], ["t1%d" % b2])
                        tt("dve", t2[b2], PS[pb][:, :], sg[1], ALU.mult, ["ps%d" % pb, "sg1"], ["t2%d" % b2])
                        tt("pool", combT[:, c, ts_], t1[b2], t2[b2], ALU.add, ["t1%d" % b2, "t2%d" % b2], ["combT"])
            S.barrier()
            if DEBUG and s == 0:
                dma("sp", dbg["combT"], VW(C_OFF, 32 * K, BF16), reads=["combT"])
                S.barrier()

            if stop("p3a"):
                break
            o_[0] = D_OFF + 4096
            wo = dalloc(16 * K, BF16).rearrange("p (k n) -> p k n", k=8)
            xts = [dalloc(4096, F32) for _ in range(2)]
            x1t = [dalloc(4096, F32) for _ in range(2)]
            xnbs = [dalloc(2048, BF16) for _ in range(2)]
            dma("pool", wo, w_out.rearrange("(k p) n -> p k n", p=128), writes=["wo"])
            for t in range(16):
                b2 = t % 2
                tks = slice(t * 128, (t + 1) * 128)
                dma("sp", xts[b2], x[s, tks, :], writes=["xts%d" % b2])
                for hh in range(2):
                    pi = 2 * b2 + hh
                    for k in range(8):
                        mm(PS[pi][:, :], combT[:, k, tks], wo[:, k, hh * 512:(hh + 1) * 512], k == 0, k == 7, ["combT", "wo"], ["ps%d" % pi])
                    tt("dve", x1t[b2][:, hh * 512:(hh + 1) * 512], PS[pi][:, :], GT(s, 0)[:, hh * 512:(hh + 1) * 512], ALU.mult,
                       ["ps%d" % pi, "GT"], ["x1t%d" % b2])
                import os as _os
                if not _os.environ.get("SKIP_ADD"):
                    tt("pool", x1t[b2], x1t[b2], xts[b2], ALU.add, ["x1t%d" % b2, "xts%d" % b2], ["x1t%d" % b2])
                if not _os.environ.get("SKIP_STORE"):
                    dma("sp", x1s[s, tks, :], x1t[b2], reads=["x1t%d" % b2], writes=["x1s"])
                if not _os.environ.get("SKIP_NORM"):
                    norm_to_T(x1t[b2], "x1t%d" % b2, hT, A2, 24, s, t, xnbs[b2], "xnbs%d" % b2, 6 + b2)
            S.barrier()
            if DEBUG and s == 0:
                dma("sp", dbg["h2T"], VW(A_OFF, 32 * K, BF16), reads=["dstT"])
                S.barrier()

            if stop("p3b"):
                break
            o_[0] = D_OFF
            wu = [dalloc(4096, BF16).rearrange("p (k n) -> p k n", k=8) for _ in range(2)]
            ub3 = [dalloc(8224, F32) for _ in range(3)]
            cvb = [dalloc(8192, F32) for _ in range(2)]
            w_up_v = w_up.rearrange("(k p) n -> p k n", p=128)
            for st_ in range(3):
                S.op("pool", lambda e, a=ub3[st_]: e.memset(a[:, 0:8], 0.0), writes=["ub%d" % st_])
            for m in range(22):
                b2 = m % 2
                dma("pool", wu[b2][:, :, 0:128], w_up_v[:, :, 128 * m:128 * m + 128], writes=["wu%d" % b2])
                dma("pool", wu[b2][:, :, 128:256], w_up_v[:, :, DFF + 128 * m:DFF + 128 * m + 128], reads=[], writes=["wu%da" % b2])
                for vg in range(2):
                    ui = (2 * m + vg) % 3
                    un = "ub%d" % ui
                    for tq in range(4):
                        zi = zbank()
                        for k in range(8):
                            mm(PS[zi][:, :], wu[b2][:, k, vg * 128:(vg + 1) * 128], hT[:, k, tq * 512:(tq + 1) * 512], k == 0, k == 7,
                               ["wu%d" % b2, "wu%da" % b2, "hT"], ["ps%d" % zi])
                        cp("act", ub3[ui][:, 8 + tq * 512:8 + (tq + 1) * 512], PS[zi][:, :], ["ps%d" % zi], [un])
                    ch = m + 22 * vg
                    u = ub3[ui]
                    cv = cvb[vg]
                    cn = "cv%d" % vg
                    tsc("pool", cv, u[:, 8:8 + 2048], wconv[:, ch, 2:3], bconv[:, ch:ch + 1], ALU.mult, ALU.add, [un, "wconv", "bconv"], [cn])
                    stt(cv, u[:, 7:7 + 2048], wconv[:, ch, 1:2], cv, ALU.mult, ALU.add, [un, cn, "wconv"], [cn])
                    stt(cv, u[:, 6:6 + 2048], wconv[:, ch, 0:1], cv, ALU.mult, ALU.add, [un, cn, "wconv"], [cn])
                act(cvb[1], cvb[1], AF.Gelu_apprx_tanh, ["cv1"], ["cv1"])
                tt("dve", yT[:, m, :], cvb[1], cvb[0], ALU.mult, ["cv0", "cv1"], ["yT"])
            S.barrier()
            if DEBUG and s == 0:
                dma("sp", dbg["yT"], VW(Y_OFF, 88 * K, BF16), reads=["yT"])
                S.barrier()

            if stop("p4"):
                break
            o_[0] = D_OFF + 4096
            wd = dalloc(44 * K, BF16).rearrange("p (k n) -> p k n", k=22)
            gfin = VW(A_OFF + 16384, 4096, F32)
            xq = [VW(A_OFF + 20480 + i * 4096, 4096, F32) for i in range(2)]
            x2t = [VW(A_OFF + i * 4096, 4096, F32) for i in range(2)]
            ot = [VW(A_OFF + 8192 + i * 4096, 4096, F32) for i in range(2)]
            dma("pool", wd, w_down.rearrange("(k p) n -> p k n", p=128), writes=["wd"])
            dma("sp", gfin, g_fin[0].partition_broadcast(128), writes=["gfin"])
            for t in range(16):
                b2 = t % 2
                tks = slice(t * 128, (t + 1) * 128)
                dma("sp", xq[b2], x1s[s, tks, :], reads=["x1s"], writes=["xq%d" % b2])
                for hh in range(2):
                    pi = 2 * b2 + hh
                    for k in range(22):
                        mm(PS[pi][:, :], yT[:, k, tks], wd[:, k, hh * 512:(hh + 1) * 512], k == 0, k == 21, ["yT", "wd"], ["ps%d" % pi])
                    tt("dve", x2t[b2][:, hh * 512:(hh + 1) * 512], PS[pi][:, :], GT(s, 1)[:, hh * 512:(hh + 1) * 512], ALU.mult,
                       ["ps%d" % pi, "GT"], ["x2t%d" % b2])
                tt("pool", x2t[b2], x2t[b2], xq[b2], ALU.add, ["x2t%d" % b2, "xq%d" % b2], ["x2t%d" % b2])
                rstd = rms_tile(x2t[b2], "x2t%d" % b2, None, None)
                stt(ot[b2], x2t[b2], rstd, gfin, ALU.mult, ALU.mult, ["x2t%d" % b2, "rstd", "gfin"], ["ot%d" % b2])
                dma("sp", out[s, tks, :], ot[b2], reads=["ot%d" % b2], writes=["out"])
            S.barrier()

        S.emit()
    return nc


_NC_CACHE = {}


def kernel(x, c, w_ada, b_ada, g_mix, w_in, w_br_moba, w_br_sb, w_out, rel_bias,
           g_ffn, w_up, w_conv, b_conv, w_down, g_final):
    f = lambda a: np.ascontiguousarray(np.asarray(a, dtype=np.float32))
    x = f(x); c = f(c)
    w_ada = f(w_ada)[0]; b_ada = f(b_ada)[0]; g_mix = f(g_mix)[0]; w_in = f(w_in)[0]
    w_bra = f(w_br_moba)[0]; w_brb = f(w_br_sb)[0]; w_out = f(w_out)[0]; rel_bias = f(rel_bias)
    g_ffn = f(g_ffn)[0]; w_up = f(w_up)[0]; w_conv = f(w_conv)[0]; b_conv = f(b_conv)[0]
    w_down = f(w_down)[0]; g_final = f(g_final)
    k = _consts()
    shared = {
        "w_ada": w_ada,
        "b_adaT": f(b_ada.reshape(48, 128).T),
        "b_ada_row": f(np.concatenate([b_ada[2048:3072], b_ada[5120:6144]])[None, :]),
        "g_mixT": f(g_mix.reshape(8, 128).T),
        "g_ffnT": f(g_ffn.reshape(8, 128).T),
        "g_fin": f(g_final[None, :]),
        "w_in": w_in, "w_bra": w_bra, "w_brb": w_brb, "w_out": w_out, "w_up": w_up,
        "w_convT": f(w_conv.T.reshape(44, 128, 3).transpose(1, 0, 2)),
        "b_convT": f(b_conv.reshape(44, 128).T),
        "w_down": w_down,
        "relG": f(rel_bias[:, k["bidx"]]),
        "b31": f(np.broadcast_to(rel_bias[:, 31][None, :], (128, 8))),
        "k_mmask": k["mmask"], "k_mpast": k["mpast"], "k_negm": k["negm"], "k_ident": k["ident"],
        "k_ustrict": k["ustrict"], "k_sel": k["sel"], "k_inv": k["inv"], "k_selrow": k["selrow"],
    }
    in_maps = []
    for i in range(NCORES):
        m = dict(shared)
        m["x"] = f(x[NSEQ * i:NSEQ * (i + 1)])
        m["cT"] = f(c[NSEQ * i:NSEQ * (i + 1)].reshape(NSEQ, 8, 128).transpose(2, 1, 0))
        in_maps.append(m)
    if "nc" not in _NC_CACHE:
        _NC_CACHE["nc"] = build()
    nc = _NC_CACHE["nc"]
    res = run_bass_kernel_spmd(nc, in_maps, core_ids=list(range(NCORES)))
    kernel.last_results = res
    return np.concatenate([np.asarray(r["out"]) for r in res.results], axis=0).astype(np.float32)
```

```python
import contextlib
import numpy as np
import concourse.bass as bass
import concourse.mybir as mybir
from concourse.bass_utils import run_bass_kernel_spmd

F32 = mybir.dt.float32
BF16 = mybir.dt.bfloat16
AF = mybir.ActivationFunctionType
ALU = mybir.AluOpType
AX = mybir.AxisListType

NCORES = 8
SEQ = 2048
D = 1024
NSEQ = 2
DFF = 2816
NEG = -30000.0

DEBUG = False

NDMASEM = 20


class Buf:
    __slots__ = ("name", "w", "r", "rd")

    def __init__(self, name):
        self.name = name
        self.w = None
        self.r = {}
        self.rd = []


class Op:
    __slots__ = ("eng", "fn", "deps", "signal", "semval", "dma", "dsem", "prewait")

    def __init__(self, eng, fn, dma):
        self.eng = eng
        self.fn = fn
        self.deps = []
        self.signal = False
        self.semval = 0
        self.dma = dma
        self.dsem = None
        self.prewait = None


class Sched:
    ENGS = ("pe", "act", "dve", "pool", "sp")

    def __init__(self, nc):
        self.nc = nc
        self.ops = {e: [] for e in self.ENGS}
        self.ndma = {e: 0 for e in self.ENGS}
        self.bufs = {}

    def _B(self, x):
        b = self.bufs.get(x)
        if b is None:
            b = Buf(x)
            self.bufs[x] = b
        return b

    def op(self, eng, fn, reads=(), writes=(), dma=False):
        o = Op(eng, fn, dma)
        deps = []
        for b in reads:
            b = self._B(b)
            if b.w is not None:
                deps.append(b.w)
            if b.name.startswith("ps"):
                for e2, r2 in b.r.items():
                    if e2 != eng:
                        deps.append(r2)
        for b in writes:
            b = self._B(b)
            if b.w is not None:
                deps.append(b.w)
            deps.extend(b.r.values())
            deps.extend(b.rd)
        for b in writes:
            b = self._B(b)
            b.w = o
            b.r = {}
            b.rd = []
        for b in reads:
            b = self._B(b)
            if b.w is not o:
                if dma:
                    b.rd.append(o)
                else:
                    b.r[eng] = o
        seen = set()
        for d in deps:
            if d is o or id(d) in seen:
                continue
            if d.eng == "pe" and eng == "pe" and not d.dma and not dma:
                continue
            seen.add(id(d))
            o.deps.append(d)
            d.signal = True
        if dma:
            k = self.ndma[eng]
            self.ndma[eng] = k + 1
            o.dsem = (eng, k % NDMASEM)
            o.semval = 16 * (k // NDMASEM + 1)
            if k >= NDMASEM:
                o.prewait = (o.dsem, 16 * (k // NDMASEM))
        self.ops[eng].append(o)
        return o

    def barrier(self):
        lasts = []
        for e in self.ENGS:
            for o in reversed(self.ops[e]):
                if not o.dma and o.fn is not None:
                    lasts.append(o)
                    break
            seen = set()
            for o in reversed(self.ops[e]):
                if o.dma and o.dsem not in seen:
                    seen.add(o.dsem)
                    lasts.append(o)
                    if len(seen) >= NDMASEM:
                        break
        for e in self.ENGS:
            o = Op(e, None, False)
            for d in lasts:
                if d.eng == e and not d.dma:
                    continue
                o.deps.append(d)
                d.signal = True
            self.ops[e].append(o)
        for b in self.bufs.values():
            b.w = None
            b.r = {}
            b.rd = []

    def emit(self):
        nc = self.nc
        for e in self.ENGS:
            c = 0
            for o in self.ops[e]:
                if o.dma:
                    continue
                if o.signal:
                    c += 1
                    o.semval = c
        with contextlib.ExitStack() as st:
            esem = {e: st.enter_context(nc.semaphore("s_" + e)) for e in self.ENGS}
            dsem = {}
            for e in self.ENGS:
                for i in range(min(NDMASEM, self.ndma[e])):
                    dsem[(e, i)] = st.enter_context(nc.semaphore("d_%s_%d" % (e, i)))
            block = st.enter_context(nc.Block())

            def run(ename, eng):
                waited = {}

                def wait(key, sem, val):
                    if waited.get(key, 0) >= val:
                        return
                    waited[key] = val
                    eng.wait_ge(sem, val)

                for o in self.ops[ename]:
                    if o.prewait is not None:
                        ds, v = o.prewait
                        wait(ds, dsem[ds], v)
                    for d in o.deps:
                        if d.dma:
                            wait(d.dsem, dsem[d.dsem], d.semval)
                        else:
                            wait(d.eng, esem[d.eng], d.semval)
                    if o.fn is None:
                        continue
                    ins = o.fn(eng)
                    if o.dma:
                        ins.then_inc(dsem[o.dsem], 16)
                    elif o.signal:
                        ins.then_inc(esem[ename], 1)

            @block.tensor
            def _(pe):
                run("pe", pe)

            @block.scalar
            def _(act):
                run("act", act)

            @block.vector
            def _(dve):
                run("dve", dve)

            @block.gpsimd
            def _(pool):
                run("pool", pool)

            @block.sync
            def _(sp):
                run("sp", sp)


def _bucket_table(nmax):
    n = np.arange(nmax, dtype=np.float64)
    nf = np.maximum(n, 16.0)
    large = 16 + np.floor(np.log(nf / 16.0) / np.log(8.0) * 16.0 + 1e-9).astype(np.int64)
    large = np.minimum(large, 31)
    return np.where(n < 16, n.astype(np.int64), large)


def _consts():
    i = np.arange(128)[:, None]
    c = np.arange(1024)[None, :]
    rel = c - 384 - i
    bt = _bucket_table(1024)
    bidx = bt[np.maximum(rel, 0)]
    mmask = np.where(rel < 0, 8.0 * NEG, 0.0).astype(np.float32)
    mpast = (rel > 0).astype(np.float32)
    negm = np.where(rel > 0, 0.0, NEG).astype(np.float32)
    ident = np.eye(128, dtype=np.float32)
    ustrict = (np.arange(128)[:, None] > np.arange(128)[None, :]).astype(np.float32)
    sel = np.zeros((16, 16, 128), np.float32)
    for w in range(16):
        sel[w, w, :] = 1.0
    sel = sel.reshape(16, 16 * 128)
    inv = np.zeros((16, 2, 8), np.float32)
    for t in range(16):
        inv[t, :, (t // 2):] = 1.0
    inv = np.broadcast_to(inv.reshape(1, 256), (128, 256)).copy()
    selrow = np.zeros((2, 2, 128), np.float32)
    selrow[0, 0, :] = 1.0
    selrow[1, 1, :] = 1.0
    selrow = selrow.reshape(2, 256)
    return dict(bidx=bidx, mmask=mmask, mpast=mpast, negm=negm, ident=ident, ustrict=ustrict,
                sel=sel, inv=inv, selrow=selrow)


def build(stop_after=None):
    nc = bass.Bass("TRN2", target_bir_lowering=False)

    def din(name, shape, dt=F32):
        return nc.dram_tensor(name, list(shape), dt, kind="ExternalInput").ap()

    x = din("x", [NSEQ, SEQ, D])
    cT = din("cT", [128, 8, 2])
    w_ada = din("w_ada", [D, 6 * D])
    b_adaT = din("b_adaT", [128, 48])
    b_ada_row = din("b_ada_row", [1, 2048])
    g_mixT = din("g_mixT", [128, 8])
    g_ffnT = din("g_ffnT", [128, 8])
    g_fin = din("g_fin", [1, D])
    w_in = din("w_in", [D, 5120])
    w_bra = din("w_bra", [512, D])
    w_brb = din("w_brb", [512, D])
    w_out = din("w_out", [D, D])
    w_up = din("w_up", [D, 2 * DFF])
    w_convT = din("w_convT", [128, 44, 3])
    b_convT = din("b_convT", [128, 44])
    w_down = din("w_down", [DFF, D])
    relG = din("relG", [8, 128, 1024])
    b31 = din("b31", [128, 8])
    k_mmask = din("k_mmask", [128, 1024])
    k_mpast = din("k_mpast", [128, 1024])
    k_negm = din("k_negm", [128, 1024])
    k_ident = din("k_ident", [128, 128])
    k_ustrict = din("k_ustrict", [128, 128])
    k_sel = din("k_sel", [16, 2048])
    k_inv = din("k_inv", [128, 256])
    k_selrow = din("k_selrow", [2, 256])
    out = nc.dram_tensor("out", [NSEQ, SEQ, D], F32, kind="ExternalOutput").ap()
    x1s = nc.dram_tensor("x1s", [NSEQ, SEQ, D], F32, kind="Internal").ap()
    dbg = {}
    if DEBUG:
        dbg["hT"] = nc.dram_tensor("dbg_hT", [128, 8 * SEQ], BF16, kind="ExternalOutput").ap()
        dbg["oT"] = nc.dram_tensor("dbg_oT", [128, 8 * SEQ], BF16, kind="ExternalOutput").ap()
        dbg["combT"] = nc.dram_tensor("dbg_combT", [128, 8 * SEQ], BF16, kind="ExternalOutput").ap()
        dbg["h2T"] = nc.dram_tensor("dbg_h2T", [128, 8 * SEQ], BF16, kind="ExternalOutput").ap()
        dbg["yT"] = nc.dram_tensor("dbg_yT", [128, 22 * SEQ], BF16, kind="ExternalOutput").ap()
        dbg["misc"] = nc.dram_tensor("dbg_misc", [128, 1024], F32, kind="ExternalOutput").ap()

    K = 1024
    TOTAL = 207 * K
    with contextlib.ExitStack() as st:
        big = st.enter_context(nc.sbuf_tensor("big", [128, TOTAL // 4], F32))
        PS = [st.enter_context(nc.psum_tensor("ps%d" % i, [128, 512], F32)) for i in range(8)]
        S = Sched(nc)

        def VW(off, nbytes, dt=F32, parts=128):
            assert off % 4 == 0 and nbytes % 4 == 0 and off + nbytes <= TOTAL, (off, nbytes)
            v = big[0:parts, off // 4:(off + nbytes) // 4]
            if dt == BF16:
                v = v.bitcast(BF16)
            return v

        def psb(i):
            return PS[i][:, :].bitcast(BF16)

        A_OFF = 0
        B_OFF = 32 * K
        C_OFF = 64 * K
        Y_OFF = 32 * K
        D_OFF = 120 * K
        D_SZ = 71 * K
        CO = 191 * K

        hT = VW(A_OFF, 32 * K, BF16).rearrange("p (k n) -> p k n", k=8)
        oT = VW(B_OFF, 32 * K, BF16).rearrange("p (k n) -> p k n", k=8)
        combT = VW(C_OFF, 32 * K, BF16).rearrange("p (k n) -> p k n", k=8)
        Bh = VW(C_OFF, 16 * K, BF16).rearrange("p (h n) -> p h n", h=8)
        yT = VW(Y_OFF, 88 * K, BF16).rearrange("p (k n) -> p k n", k=22)

        co = [CO]

        def calloc(nbytes, dt=F32, parts=128):
            v = VW(co[0], nbytes, dt, parts)
            co[0] += (nbytes + 31) // 32 * 32
            assert co[0] <= TOTAL
            return v

        identb = calloc(256, BF16)
        ustrict = calloc(256, BF16)
        onesb = calloc(256, BF16)
        onesf = calloc(512, F32)
        selb = calloc(4096, BF16)
        selrow = calloc(1024, F32)
        mpast = calloc(2048, BF16)
        negm = calloc(2048, BF16)
        inv = calloc(1024, F32)
        neginv = calloc(1024, F32)
        b31s = calloc(32, F32)
        modT = calloc(384, F32).rearrange("p (c b) -> p c b", b=2)
        A1 = calloc(64, F32).rearrange("p (c b) -> p c b", b=2)
        A2 = calloc(64, F32).rearrange("p (c b) -> p c b", b=2)
        gmixT = calloc(32, F32)
        gffnT = calloc(32, F32)
        wconv = calloc(44 * 3 * 4, F32).rearrange("p (c i) -> p c i", i=3)
        bconv = calloc(44 * 4, F32)
        badaT = calloc(48 * 4, F32)
        small = calloc(256, F32)
        CONST_END = co[0]

        def dma(eng, out_, in_, reads=(), writes=()):
            return S.op(eng, lambda e: e.dma_start(out=out_, in_=in_), reads=reads, writes=writes, dma=True)

        def mm(o, l, r, start, stop, reads, writes):
            return S.op("pe", lambda e: e.matmul(o, lhsT=l, rhs=r, start=start, stop=stop), reads=reads, writes=writes)

        def act(o, i, func, reads, writes, bias=None, scale=None, accum=None):
            kw = {}
            if bias is not None:
                kw["bias"] = bias
            if scale is not None:
                kw["scale"] = scale
            if accum is not None:
                kw["accum_out"] = accum
            return S.op("act", lambda e: e.activation(out=o, in_=i, func=func, **kw), reads=reads, writes=writes)

        def tsc(eng, o, i, s1, s2, op0, op1, reads, writes):
            if s2 is None:
                return S.op(eng, lambda e: e.tensor_scalar(out=o, in0=i, scalar1=s1, scalar2=None, op0=op0), reads=reads, writes=writes)
            return S.op(eng, lambda e: e.tensor_scalar(out=o, in0=i, scalar1=s1, scalar2=s2, op0=op0, op1=op1), reads=reads, writes=writes)

        def tt(eng, o, a, b, op, reads, writes):
            return S.op(eng, lambda e: e.tensor_tensor(out=o, in0=a, in1=b, op=op), reads=reads, writes=writes)

        def stt(o, a, s, b, op0, op1, reads, writes, accum=None):
            if accum is None:
                return S.op("dve", lambda e: e.scalar_tensor_tensor(out=o, in0=a, scalar=s, in1=b, op0=op0, op1=op1), reads=reads, writes=writes)
            return S.op("dve", lambda e: e.scalar_tensor_tensor(out=o, in0=a, scalar=s, in1=b, op0=op0, op1=op1, accum_out=accum), reads=reads, writes=writes)

        def cp(eng, o, i, reads, writes):
            if eng == "act":
                return S.op("act", lambda e: e.activation(out=o, in_=i, func=AF.Identity), reads=reads, writes=writes)
            return S.op(eng, lambda e: e.tensor_copy(out=o, in_=i), reads=reads, writes=writes)

        stg = VW(D_OFF, 4096, F32)
        stg2 = VW(D_OFF + 4096, 4096, F32)
        dma("sp", stg[:, 0:128], k_ident, writes=["stg"])
        cp("dve", identb, stg[:, 0:128], ["stg"], ["identb"])
        dma("sp", stg[:, 128:256], k_ustrict, writes=["stgb"])
        cp("dve", ustrict, stg[:, 128:256], ["stgb"], ["ustrict"])
        S.op("pool", lambda e: e.memset(onesb, 1.0), writes=["onesb"])
        S.op("pool", lambda e: e.memset(onesf, 1.0), writes=["onesf"])
        dma("pool", selb[0:16, :], k_sel, writes=["selb"])
        dma("sp", selrow[0:2, :], k_selrow, writes=["selrow"])
        dma("pool", mpast, k_mpast, writes=["mpast"])
        dma("pool", negm, k_negm, writes=["negm"])
        dma("sp", inv, k_inv, writes=["inv"])
        tsc("dve", neginv, inv, -1e30, None, ALU.mult, None, ["inv"], ["neginv"])
        dma("sp", b31s, b31, writes=["b31s"])
        dma("sp", gmixT, g_mixT, writes=["gmixT"])
        dma("sp", gffnT, g_ffnT, writes=["gffnT"])
        dma("sp", wconv, w_convT, writes=["wconv"])
        dma("sp", bconv, b_convT, writes=["bconv"])
        dma("sp", badaT, b_adaT, writes=["badaT"])

        cTt = VW(D_OFF + 8192, 64, F32).rearrange("p (k b) -> p k b", b=2)
        cactb = VW(D_OFF + 8192 + 64, 32, BF16).rearrange("p (k b) -> p k b", b=2)
        cactf = VW(D_OFF + 8192 + 128, 64, F32).rearrange("p (k b) -> p k b", b=2)
        dma("sp", cTt, cT, writes=["cTt"])
        act(cactb, cTt, AF.Silu, ["cTt"], ["cactb"])
        act(cactf, cTt, AF.Silu, ["cTt"], ["cactf"])
        gtrow = VW(D_OFF + 8192 + 256, 8192, F32, parts=2)
        badar = VW(D_OFF + 8192 + 256 + 8192, 8192, F32, parts=2)
        dma("sp", badar[0:1, :], b_ada_row, writes=["badar0"])
        dma("sp", badar[1:2, :], b_ada_row, writes=["badar1"])
        WB_OFF = D_OFF + 32 * K
        wada_v = w_ada.rearrange("(k p) n -> p k n", p=128)
        for blk in range(8):
            wb = VW(WB_OFF + (blk % 2) * 12 * K, 12 * K, BF16).rearrange("p (k n) -> p k n", k=8)
            wn = "wada%d" % (blk % 2)
            dma("pool", wb, wada_v[:, :, blk * 768:(blk + 1) * 768], writes=[wn])
            for j in range(6):
                cidx = blk * 6 + j
                for k in range(8):
                    mm(PS[0][:, 2 * cidx:2 * cidx + 2], wb[:, k, j * 128:(j + 1) * 128], cactb[:, k, :], k == 0, k == 7,
                       [wn, "cactb"], ["ps0"])
        tt("dve", modT, PS[0][:, 0:96].rearrange("p (c b) -> p c b", b=2),
           badaT.rearrange("p (c o) -> p c o", o=1).to_broadcast([128, 48, 2]), ALU.add, ["ps0", "badaT"], ["modT"])
        tsc("dve", A1, modT[:, 8:16, :], 1.0, None, ALU.add, None, ["modT"], ["A1"])
        tt("dve", A1, A1, gmixT.rearrange("p (c o) -> p c o", o=1).to_broadcast([128, 8, 2]), ALU.mult, ["A1", "gmixT"], ["A1"])
        tsc("dve", A2, modT[:, 32:40, :], 1.0, None, ALU.add, None, ["modT"], ["A2"])
        tt("dve", A2, A2, gffnT.rearrange("p (c o) -> p c o", o=1).to_broadcast([128, 8, 2]), ALU.mult, ["A2", "gffnT"], ["A2"])
        GT_OFF = D_OFF + 32 * K + 24 * K
        S.barrier()
        wf = VW(D_OFF + 26 * K, 32 * K, F32).rearrange("p (k n) -> p k n", k=8)
        for g, c0 in enumerate((2048, 5120)):
            dma("sp", wf, wada_v[:, :, c0:c0 + 1024], writes=["wf"])
            for hh in range(2):
                for k in range(8):
                    mm(PS[1][0:2, :], cactf[:, k, :], wf[:, k, hh * 512:(hh + 1) * 512], k == 0, k == 7, ["wf", "cactf"], ["ps1"])
                tt("dve", gtrow[0:2, g * 1024 + hh * 512:g * 1024 + (hh + 1) * 512], PS[1][0:2, :],
                   badar[0:2, g * 1024 + hh * 512:g * 1024 + (hh + 1) * 512], ALU.add, ["ps1", "badar0", "badar1"], ["gtrow"])
        GTB = D_OFF + D_SZ - 16 * K

        def GT(s, g):
            return VW(GTB + (s * 2 + g) * 4096, 4096, F32)

        for s in range(NSEQ):
            for g in range(2):
                for hh in range(2):
                    mm(PS[2][:, :], selrow[0:2, s * 128:(s + 1) * 128], gtrow[0:2, g * 1024 + hh * 512:g * 1024 + (hh + 1) * 512],
                       True, True, ["selrow", "gtrow"], ["ps2"])
                    cp("act", GT(s, g)[:, hh * 512:(hh + 1) * 512], PS[2][:, :], ["ps2"], ["GT"])
        S.barrier()
        STOP = [False]

        def stop(tag):
            if stop_after == tag:
                STOP[0] = True
            return STOP[0]
        DW = GTB - D_OFF

        def rms_tile(xt_ap, xtn, ssn, tag):
            junk = VW(D_OFF + 0, 4096, F32)
            ss = small[:, 0:1]
            lnv = small[:, 1:2]
            rstd = small[:, 2:3]
            stt(junk, xt_ap, 1.0, xt_ap, ALU.mult, ALU.mult, [xtn], ["junk", "ss"], accum=ss)
            act(lnv, ss, AF.Ln, ["ss"], ["lnv"], bias=1e-6, scale=1.0 / 1024.0)
            act(rstd, lnv, AF.Exp, ["lnv"], ["rstd"], scale=-0.5)
            return rstd

        def norm_to_T(xt_ap, xtn, dstT, Amod, Bmod_c0, s, t, xnb, xnbn, psi):
            rstd = rms_tile(xt_ap, xtn, None, None)
            tsc("dve", xnb, xt_ap, rstd, None, ALU.mult, None, [xtn, "rstd"], [xnbn])
            pn = "ps%d" % psi
            pv = psb(psi)
            for c in range(8):
                S.op("pe", lambda e, c=c: e.transpose(out=pv[:, c * 128:(c + 1) * 128], in_=xnb[:, c * 128:(c + 1) * 128], identity=identb),
                     reads=[xnbn, "identb"], writes=[pn])
            for c in range(8):
                o = dstT[:, c, t * 128:(t + 1) * 128]
                i = pv[:, c * 128:(c + 1) * 128]
                if t % 2 == 0:
                    act(o, i, AF.Identity, [pn, "A1", "A2", "modT"], ["dstT"], bias=modT[:, Bmod_c0 + c, s:s + 1], scale=Amod[:, c, s:s + 1])
                else:
                    tsc("dve", o, i, Amod[:, c, s:s + 1], modT[:, Bmod_c0 + c, s:s + 1], ALU.mult, ALU.add, [pn, "A1", "A2", "modT"], ["dstT"])

        w_in_v = w_in.rearrange("(k p) n -> p k n", p=128)

        for s in range(NSEQ):
            if stop("setup"):
                break
            for t in range(16):
                xt = VW(D_OFF + 4096 + (t % 2) * 4096, 4096, F32)
                xtn = "xt%d" % (t % 2)
                dma("sp", xt, x[s, t * 128:(t + 1) * 128, :], writes=[xtn])
                xnb = VW(D_OFF + 12288 + (t % 2) * 2048, 2048, BF16)
                norm_to_T(xt, xtn, hT, A1, 0, s, t, xnb, "xnb%d" % (t % 2), 6 + (t % 2))
            S.barrier()
            if DEBUG and s == 0:
                dma("sp", dbg["hT"], VW(A_OFF, 32 * K, BF16), reads=["dstT"])
                S.barrier()

            if stop("p1"):
                break
            mm_f = VW(D_OFF, 4096, F32)
            dma("sp", mm_f, k_mmask, writes=["mm_f"])
            for h in range(8):
                gtile = VW(D_OFF + 4096 + (h % 2) * 4096, 4096, F32)
                gn = "gt%d" % (h % 2)
                dma("sp", gtile, relG[h], writes=[gn])
                tsc("dve", gtile, gtile, b31s[:, h:h + 1], 8.0, ALU.subtract, ALU.mult, [gn, "b31s"], [gn])
                tt("dve", Bh[:, h, :], gtile, mm_f, ALU.add, [gn, "mm_f"], ["Bh"])
            S.barrier()
            o_ = [D_OFF]

            def dalloc(nbytes, dt=F32, parts=128):
                v = VW(o_[0], nbytes, dt, parts)
                o_[0] += (nbytes + 31) // 32 * 32
                assert o_[0] <= GTB, o_[0] - GTB
                return v

            wq = [dalloc(2048, BF16).rearrange("p (k n) -> p k n", k=8) for _ in range(1)]
            wk = [dalloc(2048, BF16).rearrange("p (k n) -> p k n", k=8) for _ in range(1)]
            wv = [dalloc(2048, BF16).rearrange("p (k n) -> p k n", k=8) for _ in range(1)]
            qT = dalloc(4096, BF16)
            kT = dalloc(4096, BF16)
            Vaug = dalloc(8192, BF16).rearrange("p (t n) -> p t n", t=16)
            negT = dalloc(4096, BF16)
            gm = dalloc(1024, F32)
            top8 = dalloc(1024, F32)
            mge = dalloc(1024, F32)
            negq = dalloc(512, BF16)
            kms = dalloc(32, F32)
            kmh = dalloc(16, BF16)
            kmr = dalloc(32, F32)
            kml = dalloc(16, BF16)
            PT = [dalloc(1024, BF16) for _ in range(3)]
            e_t = [dalloc(2048, F32) for _ in range(2)]
            sp_t = [dalloc(2048, F32) for _ in range(2)]
            lkm = [dalloc(1024, BF16) for _ in range(2)]
            ssum = [dalloc(1024, BF16) for _ in range(2)]
            t_t = [dalloc(2048, F32) for _ in range(2)]
            a_t = [dalloc(1024, BF16) for _ in range(2)]
            rr = e_t[0]
            lnr = e_t[1]
            bcs = sp_t[0]

            S.op("pool", lambda e: e.memset(Vaug, 0.0), writes=["Vaug"])
            S.op("pool", lambda e: e.memset(Vaug[:, :, 64:65], 1.0), reads=[], writes=["Vaug"])
            S.op("pool", lambda e: e.memset(Vaug[:, :, 128:129], 1.0), reads=[], writes=["Vaug"])

            zc = [0]

            def zbank():
                i = zc[0] % 3
                zc[0] += 1
                return i

            def project_pair(kind, p):
                base = 0 if kind == 0 else 1536
                pp = 0
                dma("pool", wq[pp], w_in_v[:, :, base + 128 * p:base + 128 * p + 128], writes=["wq%d" % pp])
                dma("pool", wk[pp], w_in_v[:, :, base + 512 + 128 * p:base + 512 + 128 * p + 128], writes=["wk%d" % pp])
                dma("pool", wv[pp], w_in_v[:, :, base + 1024 + 128 * p:base + 1024 + 128 * p + 128], writes=["wv%d" % pp])
                for (wt, wn, dst, dn) in ((wq[pp], "wq%d" % pp, qT, "qT"), (wk[pp], "wk%d" % pp, kT, "kT")):
                    for tq in range(4):
                        zi = zbank()
                        for k in range(8):
                            mm(PS[zi][:, :], wt[:, k, :], hT[:, k, tq * 512:(tq + 1) * 512], k == 0, k == 7, [wn, "hT"], ["ps%d" % zi])
                        cp("act" if tq % 2 == 0 else "dve", dst[:, tq * 512:(tq + 1) * 512], PS[zi][:, :], ["ps%d" % zi], [dn])
                for g in range(4):
                    zi = zbank()
                    for tl in range(4):
                        t = g * 4 + tl
                        for k in range(8):
                            mm(PS[zi][:, tl * 128:(tl + 1) * 128], hT[:, k, t * 128:(t + 1) * 128], wv[pp][:, k, :], k == 0, k == 7,
                               ["wv%d" % pp, "hT"], ["ps%d" % zi])
                    pv3 = PS[zi][:, :].rearrange("p (t n) -> p t n", t=4)
                    cp("act", Vaug[:, g * 4:(g + 1) * 4, 0:64], pv3[:, :, 0:64], ["ps%d" % zi], ["Vaug"])
                    cp("dve", Vaug[:, g * 4:(g + 1) * 4, 192:256], pv3[:, :, 64:128], ["ps%d" % zi], ["Vaug"])

            def moba_gate():
                tensor_reduce = lambda e: e.tensor_reduce(out=kms, in_=kT.rearrange("p (n j) -> p n j", j=256), axis=AX.X, op=ALU.add)
                S.op("dve", tensor_reduce, reads=["kT"], writes=["kms"])
                tsc("dve", kms, kms, 1.0 / 256.0, None, ALU.mult, None, ["kms"], ["kms"])
                cp("dve", kmh, kms, ["kms"], ["kmh"])
                tt("dve", kmr, kms, kmh, ALU.subtract, ["kms", "kmh"], ["kmr"])
                cp("dve", kml, kmr, ["kmr"], ["kml"])
                gm4 = gm.rearrange("p (t h n) -> p t h n", t=16, h=2)
                ni4 = neginv.rearrange("p (t h n) -> p t h n", t=16, h=2)
                for hd in range(2):
                    hs = slice(64 * hd, 64 * hd + 64)
                    gb = 3 + hd
                    gv = PS[gb][:, 0:128].rearrange("p (t n) -> p t n", t=16)
                    for t in range(16):
                        mm(gv[:, t, :], qT[hs, t * 128:(t + 1) * 128], kmh[hs, :], True, False, ["qT", "kmh"], ["ps%d" % gb])
                        mm(gv[:, t, :], qT[hs, t * 128:(t + 1) * 128], kml[hs, :], False, True, ["qT", "kml"], ["ps%d" % gb])
                    tt("dve", gm4[:, :, hd, :], gv, ni4[:, :, hd, :], ALU.add, ["ps%d" % gb, "neginv"], ["gm"])
                gm3 = gm.rearrange("p (g n) -> p g n", n=8)
                t83 = top8.rearrange("p (g n) -> p g n", n=8)
                for g in range(32):
                    S.op("dve", lambda e, g=g: e.max(out=t83[:, g, :], in_=gm3[:, g, :]), reads=["gm"], writes=["top8"])
                tt("dve", mge.rearrange("p (g n) -> p g n", n=8), gm3, t83[:, :, 2:3].to_broadcast([128, 32, 8]), ALU.is_ge,
                   ["gm", "top8"], ["mge"])
                tt("dve", mge, mge, inv, ALU.max, ["mge", "inv"], ["mge"])
                tsc("dve", negq, mge, -8.0 * NEG, 8.0 * NEG, ALU.mult, ALU.add, ["mge"], ["negq"])
                for half in range(2):
                    pv = psb(4)
                    for tl in range(8):
                        t = half * 8 + tl
                        S.op("pe", lambda e, t=t, tl=tl, pv=pv: e.transpose(out=pv[0:16, tl * 128:(tl + 1) * 128], in_=negq[:, t * 16:(t + 1) * 16], identity=identb),
                             reads=["negq", "identb"], writes=["ps4"])
                    cp("dve", negT[0:16, half * 1024:(half + 1) * 1024], pv[0:16, :], ["ps4"], ["negT"])

            acc_c = [0]

            def moba_head(p, hd):
                head = 2 * p + hd
                hs = slice(64 * hd, 64 * hd + 64)
                for qt in range(4):
                    ai = 5 + (acc_c[0] % 2)
                    acc_c[0] += 1
                    an = "ps%d" % ai
                    nch = 4 * qt + 4
                    qs = slice(qt * 512, (qt + 1) * 512)
                    for kc in range(nch):
                        zi = zbank()
                        zn = "ps%d" % zi
                        n = kc // 2
                        d0 = 512 * qt - 128 * kc
                        use_sel = n <= 2 * qt
                        use_toe = d0 < 240
                        mm(PS[zi][:, :], kT[hs, kc * 128:(kc + 1) * 128], qT[hs, qs], True, not (use_sel or use_toe), ["kT", "qT"], [zn])
                        if use_sel:
                            w = hd * 8 + n
                            mm(PS[zi][:, :], selb[0:16, w * 128:(w + 1) * 128], negT[0:16, qs], False, not use_toe, ["selb", "negT"], [zn])
                        if use_toe:
                            c0 = d0 + 384
                            mm(PS[zi][:, :], identb, Bh[:, head, c0:c0 + 512], False, True, ["identb", "Bh"], [zn])
                        pi = kc % 3
                        act(PT[pi], PS[zi][:, :], AF.Exp, [zn, "b31s"], ["PT%d" % pi], bias=b31s[:, head:head + 1], scale=0.125)
                        if hd == 0:
                            mm(PS[ai][0:65, :], Vaug[:, kc, 0:65], PT[pi], kc == 0, kc == nch - 1, ["Vaug", "PT%d" % pi], [an])
                        else:
                            mm(PS[ai][:, :], Vaug[:, kc, 128:256], PT[pi], kc == 0, kc == nch - 1, ["Vaug", "PT%d" % pi], [an])
                    srow = 64 if hd == 0 else 0
                    act(lnr[0:1, 0:512], PS[ai][srow:srow + 1, :], AF.Ln, [an], ["e1"])
                    act(rr[0:1, 0:512], lnr[0:1, 0:512], AF.Exp, ["e1"], ["e0"], scale=-1.0)
                    mm(PS[7][:, :], onesf[0:1, :], rr[0:1, 0:512], True, True, ["onesf", "e0"], ["ps7"])
                    cp("act", bcs[hs, 0:512], PS[7][hs, :], ["ps7"], ["sp0"])
                    tt("dve", oT[hs, p, qs], PS[ai][hs, :], bcs[hs, 0:512], ALU.mult, [an, "sp0"], ["oT"])

            c2 = [C_OFF + 16 * K]

            def c2alloc(nbytes, dt=F32):
                v = VW(c2[0], nbytes, dt)
                c2[0] += nbytes
                assert c2[0] <= C_OFF + 32 * K
                return v

            SBS = [dict(e=e_t, sp=sp_t, lkm=lkm, ssum=ssum, t=t_t, a=a_t, sfx="", L=3, acc=5),
                   dict(e=[c2alloc(2048) for _ in range(2)], sp=[c2alloc(2048) for _ in range(2)],
                        lkm=[c2alloc(1024, BF16) for _ in range(2)], ssum=[c2alloc(1024, BF16) for _ in range(2)],
                        t=[c2alloc(2048) for _ in range(2)], a=[dalloc(1024, BF16) for _ in range(2)], sfx="b", L=4, acc=6)]

            def sb_gen(p, hd):
                B = SBS[hd]
                sx = B["sfx"]
                hs = slice(64 * hd, 64 * hd + 64)
                li = B["L"]
                ln_ = "ps%d" % li
                ai = B["acc"]
                an = "ps%d" % ai
                for qt in range(4):
                    nch = 4 * qt + 4
                    qs = slice(qt * 512, (qt + 1) * 512)
                    for idx in range(nch):
                        kc = nch - 1 - idx
                        b2 = idx % 2
                        zi = zbank()
                        zn = "ps%d" % zi
                        d0 = 512 * qt - 128 * kc
                        diag = kc >= 4 * qt
                        c0 = d0 + 384
                        en, spn, lkn, tn, an_ = ("e%d%s" % (b2, sx), "sp%d%s" % (b2, sx), "lkm%d%s" % (b2, sx), "t%d%s" % (b2, sx), "a%d%s" % (b2, sx))
                        e_b, sp_b, lk_b, t_b, a_b = B["e"][b2], B["sp"][b2], B["lkm"][b2], B["t"][b2], B["a"][b2]
                        mm(PS[zi][:, :], kT[hs, kc * 128:(kc + 1) * 128], qT[hs, qs], True, True, ["kT", "qT"], [zn])
                        act(e_b, PS[zi][:, :], AF.Exp, [zn], [en], scale=-0.125)
                        act(sp_b, e_b, AF.Ln, [en], [spn], bias=1.0)
                        stt(lk_b, PS[zi][:, :], 0.125, sp_b, ALU.mult, ALU.add, [zn, spn], [lkn])
                        if diag:
                            tt("pool", lk_b, lk_b, mpast[:, c0:c0 + 512], ALU.mult, [lkn, "mpast"], [lkn])
                        mm(PS[li][:, :], ustrict, lk_b, True, idx == 0, ["ustrict", lkn], [ln_])
                        if idx > 0:
                            mm(PS[li][:, :], onesb, B["ssum"][(idx - 1) % 2], False, True, ["onesb", "ssum%d%s" % ((idx - 1) % 2, sx)], [ln_])
                        if kc > 0:
                            if idx == 0:
                                cp("pool", B["ssum"][0], lk_b, [lkn], ["ssum0%s" % sx])
                            else:
                                tt("pool", B["ssum"][idx % 2], B["ssum"][(idx - 1) % 2], lk_b, ALU.add,
                                   ["ssum%d%s" % ((idx - 1) % 2, sx), lkn], ["ssum%d%s" % (idx % 2, sx)])
                        stt(t_b, PS[li][:, :], -1.0, sp_b, ALU.mult, ALU.subtract, [ln_, spn], [tn])
                        if diag:
                            tt("pool", t_b, t_b, negm[:, c0:c0 + 512], ALU.add, [tn, "negm"], [tn])
                        act(a_b, t_b, AF.Exp, [tn], [an_])
                        if hd == 0:
                            mm(PS[ai][0:64, :], Vaug[:, kc, 0:64], a_b, idx == 0, kc == 0, ["Vaug", an_], [an])
                        else:
                            mm(PS[ai][:, :], Vaug[:, kc, 128:256], a_b, idx == 0, kc == 0, ["Vaug", an_], [an])
                        yield
                    cp("act", oT[hs, 4 + p, qs], PS[ai][hs, :], [an], ["oT"])

            def sb_pair(p):
                gens = [sb_gen(p, 0), sb_gen(p, 1)]
                alive = [True, True]
                while any(alive):
                    for i in range(2):
                        if alive[i]:
                            try:
                                next(gens[i])
                            except StopIteration:
                                alive[i] = False

            for kind in range(2):
                for p in range(4):
                    project_pair(kind, p)
                    if kind == 0:
                        moba_gate()
                        for hd in range(2):
                            moba_head(p, hd)
                    else:
                        sb_pair(p)
            S.barrier()
            if DEBUG and s == 0:
                dma("sp", dbg["oT"], VW(B_OFF, 32 * K, BF16), reads=["oT"])
                S.barrier()

            if stop("p2"):
                break
            o_[0] = D_OFF
            wbra = dalloc(8192, BF16).rearrange("p (k n) -> p k n", k=4)
            wbrb = dalloc(8192, BF16).rearrange("p (k n) -> p k n", k=4)
            wg = dalloc(16 * K, BF16).rearrange("p (k n) -> p k n", k=8)
            sg = [dalloc(2048, F32) for _ in range(2)]
            t1 = [dalloc(2048, F32) for _ in range(2)]
            t2 = [dalloc(2048, F32) for _ in range(2)]
            dma("pool", wbra, w_bra.rearrange("(k p) n -> p k n", p=128), writes=["wbra"])
            dma("pool", wbrb, w_brb.rearrange("(k p) n -> p k n", p=128), writes=["wbrb"])
            cnt = 0
            for cg in range(2):
                for j in range(4):
                    c = cg * 4 + j
                    dma("pool", wg[:, :, j * 256:j * 256 + 128], w_in_v[:, :, 3072 + 128 * c:3072 + 128 * c + 128], reads=[], writes=["wga%d" % j])
                    dma("pool", wg[:, :, j * 256 + 128:j * 256 + 256], w_in_v[:, :, 4096 + 128 * c:4096 + 128 * c + 128], reads=[], writes=["wgb%d" % j])
                for j in range(4):
                    c = cg * 4 + j
                    for tq in range(4):
                        ts_ = slice(tq * 512, (tq + 1) * 512)
                        b2 = cnt % 2
                        cnt += 1
                        pa, pb, pga, pgb = (0, 1, 2, 3) if b2 == 0 else (4, 5, 6, 7)
                        for k in range(4):
                            mm(PS[pa][:, :], wbra[:, k, c * 128:(c + 1) * 128], oT[:, k, ts_], k == 0, k == 3, ["wbra", "oT"], ["ps%d" % pa])
                        for k in range(4):
                            mm(PS[pb][:, :], wbrb[:, k, c * 128:(c + 1) * 128], oT[:, 4 + k, ts_], k == 0, k == 3, ["wbrb", "oT"], ["ps%d" % pb])
                        for k in range(8):
                            mm(PS[pga][:, :], wg[:, k, j * 256:j * 256 + 128], hT[:, k, ts_], k == 0, k == 7, ["wga%d" % j, "hT"], ["ps%d" % pga])
                        for k in range(8):
                            mm(PS[pgb][:, :], wg[:, k, j * 256 + 128:j * 256 + 256], hT[:, k, ts_], k == 0, k == 7, ["wgb%d" % j, "hT"], ["ps%d" % pgb])
                        act(sg[0], PS[pga][:, :], AF.Sigmoid, ["ps%d" % pga], ["sg0"])
                        act(sg[1], PS[pgb][:, :], AF.Sigmoid, ["ps%d" % pgb], ["sg1"])
                        tt("dve", t1[b2], PS[pa][:, :], sg[0], ALU.mult, ["ps%d" % pa, "sg0"], ["t1%d" % b2])
                        tt("dve", t2[b2], PS[pb][:, :], sg[1], ALU.mult, ["ps%d" % pb, "sg1"], ["t2%d" % b2])
                        tt("pool", combT[:, c, ts_], t1[b2], t2[b2], ALU.add, ["t1%d" % b2, "t2%d" % b2], ["combT"])
            S.barrier()
            if DEBUG and s == 0:
                dma("sp", dbg["combT"], VW(C_OFF, 32 * K, BF16), reads=["combT"])
                S.barrier()

            if stop("p3a"):
                break
            o_[0] = D_OFF + 4096
            wo = dalloc(16 * K, BF16).rearrange("p (k n) -> p k n", k=8)
            xts = [dalloc(4096, F32) for _ in range(2)]
            x1t = [dalloc(4096, F32) for _ in range(2)]
            xnbs = [dalloc(2048, BF16) for _ in range(2)]
            dma("pool", wo, w_out.rearrange("(k p) n -> p k n", p=128), writes=["wo"])
            for t in range(16):
                b2 = t % 2
                tks = slice(t * 128, (t + 1) * 128)
                dma("sp", xts[b2], x[s, tks, :], writes=["xts%d" % b2])
                for hh in range(2):
                    pi = 2 * b2 + hh
                    for k in range(8):
                        mm(PS[pi][:, :], combT[:, k, tks], wo[:, k, hh * 512:(hh + 1) * 512], k == 0, k == 7, ["combT", "wo"], ["ps%d" % pi])
                    tt("dve", x1t[b2][:, hh * 512:(hh + 1) * 512], PS[pi][:, :], GT(s, 0)[:, hh * 512:(hh + 1) * 512], ALU.mult,
                       ["ps%d" % pi, "GT"], ["x1t%d" % b2])
                import os as _os
                if not _os.environ.get("SKIP_ADD"):
                    tt("pool", x1t[b2], x1t[b2], xts[b2], ALU.add, ["x1t%d" % b2, "xts%d" % b2], ["x1t%d" % b2])
                if not _os.environ.get("SKIP_STORE"):
                    dma("sp", x1s[s, tks, :], x1t[b2], reads=["x1t%d" % b2], writes=["x1s"])
                if not _os.environ.get("SKIP_NORM"):
                    norm_to_T(x1t[b2], "x1t%d" % b2, hT, A2, 24, s, t, xnbs[b2], "xnbs%d" % b2, 6 + b2)
            S.barrier()
            if DEBUG and s == 0:
                dma("sp", dbg["h2T"], VW(A_OFF, 32 * K, BF16), reads=["dstT"])
                S.barrier()

            if stop("p3b"):
                break
            o_[0] = D_OFF
            wu = [dalloc(4096, BF16).rearrange("p (k n) -> p k n", k=8) for _ in range(2)]
            ub3 = [dalloc(8224, F32) for _ in range(3)]
            cvb = [dalloc(8192, F32) for _ in range(2)]
            w_up_v = w_up.rearrange("(k p) n -> p k n", p=128)
            for st_ in range(3):
                S.op("pool", lambda e, a=ub3[st_]: e.memset(a[:, 0:8], 0.0), writes=["ub%d" % st_])
            for m in range(22):
                b2 = m % 2
                dma("pool", wu[b2][:, :, 0:128], w_up_v[:, :, 128 * m:128 * m + 128], writes=["wu%d" % b2])
                dma("pool", wu[b2][:, :, 128:256], w_up_v[:, :, DFF + 128 * m:DFF + 128 * m + 128], reads=[], writes=["wu%da" % b2])
                for vg in range(2):
                    ui = (2 * m + vg) % 3
                    un = "ub%d" % ui
                    for tq in range(4):
                        zi = zbank()
                        for k in range(8):
                            mm(PS[zi][:, :], wu[b2][:, k, vg * 128:(vg + 1) * 128], hT[:, k, tq * 512:(tq + 1) * 512], k == 0, k == 7,
                               ["wu%d" % b2, "wu%da" % b2, "hT"], ["ps%d" % zi])
                        cp("act", ub3[ui][:, 8 + tq * 512:8 + (tq + 1) * 512], PS[zi][:, :], ["ps%d" % zi], [un])
                    ch = m + 22 * vg
                    u = ub3[ui]
                    cv = cvb[vg]
                    cn = "cv%d" % vg
                    tsc("pool", cv, u[:, 8:8 + 2048], wconv[:, ch, 2:3], bconv[:, ch:ch + 1], ALU.mult, ALU.add, [un, "wconv", "bconv"], [cn])
                    stt(cv, u[:, 7:7 + 2048], wconv[:, ch, 1:2], cv, ALU.mult, ALU.add, [un, cn, "wconv"], [cn])
                    stt(cv, u[:, 6:6 + 2048], wconv[:, ch, 0:1], cv, ALU.mult, ALU.add, [un, cn, "wconv"], [cn])
                act(cvb[1], cvb[1], AF.Gelu_apprx_tanh, ["cv1"], ["cv1"])
                tt("dve", yT[:, m, :], cvb[1], cvb[0], ALU.mult, ["cv0", "cv1"], ["yT"])
            S.barrier()
            if DEBUG and s == 0:
                dma("sp", dbg["yT"], VW(Y_OFF, 88 * K, BF16), reads=["yT"])
                S.barrier()

            if stop("p4"):
                break
            o_[0] = D_OFF + 4096
            wd = dalloc(44 * K, BF16).rearrange("p (k n) -> p k n", k=22)
            gfin = VW(A_OFF + 16384, 4096, F32)
            xq = [VW(A_OFF + 20480 + i * 4096, 4096, F32) for i in range(2)]
            x2t = [VW(A_OFF + i * 4096, 4096, F32) for i in range(2)]
            ot = [VW(A_OFF + 8192 + i * 4096, 4096, F32) for i in range(2)]
            dma("pool", wd, w_down.rearrange("(k p) n -> p k n", p=128), writes=["wd"])
            dma("sp", gfin, g_fin[0].partition_broadcast(128), writes=["gfin"])
            for t in range(16):
                b2 = t % 2
                tks = slice(t * 128, (t + 1) * 128)
                dma("sp", xq[b2], x1s[s, tks, :], reads=["x1s"], writes=["xq%d" % b2])
                for hh in range(2):
                    pi = 2 * b2 + hh
                    for k in range(22):
                        mm(PS[pi][:, :], yT[:, k, tks], wd[:, k, hh * 512:(hh + 1) * 512], k == 0, k == 21, ["yT", "wd"], ["ps%d" % pi])
                    tt("dve", x2t[b2][:, hh * 512:(hh + 1) * 512], PS[pi][:, :], GT(s, 1)[:, hh * 512:(hh + 1) * 512], ALU.mult,
                       ["ps%d" % pi, "GT"], ["x2t%d" % b2])
                tt("pool", x2t[b2], x2t[b2], xq[b2], ALU.add, ["x2t%d" % b2, "xq%d" % b2], ["x2t%d" % b2])
                rstd = rms_tile(x2t[b2], "x2t%d" % b2, None, None)
                stt(ot[b2], x2t[b2], rstd, gfin, ALU.mult, ALU.mult, ["x2t%d" % b2, "rstd", "gfin"], ["ot%d" % b2])
                dma("sp", out[s, tks, :], ot[b2], reads=["ot%d" % b2], writes=["out"])
            S.barrier()

        S.emit()
    return nc


_NC_CACHE = {}


def kernel(x, c, w_ada, b_ada, g_mix, w_in, w_br_moba, w_br_sb, w_out, rel_bias,
           g_ffn, w_up, w_conv, b_conv, w_down, g_final):
    f = lambda a: np.ascontiguousarray(np.asarray(a, dtype=np.float32))
    x = f(x); c = f(c)
    w_ada = f(w_ada)[0]; b_ada = f(b_ada)[0]; g_mix = f(g_mix)[0]; w_in = f(w_in)[0]
    w_bra = f(w_br_moba)[0]; w_brb = f(w_br_sb)[0]; w_out = f(w_out)[0]; rel_bias = f(rel_bias)
    g_ffn = f(g_ffn)[0]; w_up = f(w_up)[0]; w_conv = f(w_conv)[0]; b_conv = f(b_conv)[0]
    w_down = f(w_down)[0]; g_final = f(g_final)
    k = _consts()
    shared = {
        "w_ada": w_ada,
        "b_adaT": f(b_ada.reshape(48, 128).T),
        "b_ada_row": f(np.concatenate([b_ada[2048:3072], b_ada[5120:6144]])[None, :]),
        "g_mixT": f(g_mix.reshape(8, 128).T),
        "g_ffnT": f(g_ffn.reshape(8, 128).T),
        "g_fin": f(g_final[None, :]),
        "w_in": w_in, "w_bra": w_bra, "w_brb": w_brb, "w_out": w_out, "w_up": w_up,
        "w_convT": f(w_conv.T.reshape(44, 128, 3).transpose(1, 0, 2)),
        "b_convT": f(b_conv.reshape(44, 128).T),
        "w_down": w_down,
        "relG": f(rel_bias[:, k["bidx"]]),
        "b31": f(np.broadcast_to(rel_bias[:, 31][None, :], (128, 8))),
        "k_mmask": k["mmask"], "k_mpast": k["mpast"], "k_negm": k["negm"], "k_ident": k["ident"],
        "k_ustrict": k["ustrict"], "k_sel": k["sel"], "k_inv": k["inv"], "k_selrow": k["selrow"],
    }
    in_maps = []
    for i in range(NCORES):
        m = dict(shared)
        m["x"] = f(x[NSEQ * i:NSEQ * (i + 1)])
        m["cT"] = f(c[NSEQ * i:NSEQ * (i + 1)].reshape(NSEQ, 8, 128).transpose(2, 1, 0))
        in_maps.append(m)
    if "nc" not in _NC_CACHE:
        _NC_CACHE["nc"] = build()
    nc = _NC_CACHE["nc"]
    res = run_bass_kernel_spmd(nc, in_maps, core_ids=list(range(NCORES)))
    kernel.last_results = res
    return np.concatenate([np.asarray(r["out"]) for r in res.results], axis=0).astype(np.float32)
```

```python
import contextlib
import numpy as np
import concourse.bass as bass
import concourse.mybir as mybir
from concourse.bass_utils import run_bass_kernel_spmd

F32 = mybir.dt.float32
BF16 = mybir.dt.bfloat16
AF = mybir.ActivationFunctionType
ALU = mybir.AluOpType
AX = mybir.AxisListType

NCORES = 8
SEQ = 2048
D = 1024
NSEQ = 2
DFF = 2816
NEG = -30000.0

DEBUG = False

NDMASEM = 20


class Buf:
    __slots__ = ("name", "w", "r", "rd")

    def __init__(self, name):
        self.name = name
        self.w = None
        self.r = {}
        self.rd = []


class Op:
    __slots__ = ("eng", "fn", "deps", "signal", "semval", "dma", "dsem", "prewait")

    def __init__(self, eng, fn, dma):
        self.eng = eng
        self.fn = fn
        self.deps = []
        self.signal = False
        self.semval = 0
        self.dma = dma
        self.dsem = None
        self.prewait = None


class Sched:
    ENGS = ("pe", "act", "dve", "pool", "sp")

    def __init__(self, nc):
        self.nc = nc
        self.ops = {e: [] for e in self.ENGS}
        self.ndma = {e: 0 for e in self.ENGS}
        self.bufs = {}

    def _B(self, x):
        b = self.bufs.get(x)
        if b is None:
            b = Buf(x)
            self.bufs[x] = b
        return b

    def op(self, eng, fn, reads=(), writes=(), dma=False):
        o = Op(eng, fn, dma)
        deps = []
        for b in reads:
            b = self._B(b)
            if b.w is not None:
                deps.append(b.w)
            if b.name.startswith("ps"):
                for e2, r2 in b.r.items():
                    if e2 != eng:
                        deps.append(r2)
        for b in writes:
            b = self._B(b)
            if b.w is not None:
                deps.append(b.w)
            deps.extend(b.r.values())
            deps.extend(b.rd)
        for b in writes:
            b = self._B(b)
            b.w = o
            b.r = {}
            b.rd = []
        for b in reads:
            b = self._B(b)
            if b.w is not o:
                if dma:
                    b.rd.append(o)
                else:
                    b.r[eng] = o
        seen = set()
        for d in deps:
            if d is o or id(d) in seen:
                continue
            if d.eng == "pe" and eng == "pe" and not d.dma and not dma:
                continue
            seen.add(id(d))
            o.deps.append(d)
            d.signal = True
        if dma:
            k = self.ndma[eng]
            self.ndma[eng] = k + 1
            o.dsem = (eng, k % NDMASEM)
            o.semval = 16 * (k // NDMASEM + 1)
            if k >= NDMASEM:
                o.prewait = (o.dsem, 16 * (k // NDMASEM))
        self.ops[eng].append(o)
        return o

    def barrier(self):
        lasts = []
        for e in self.ENGS:
            for o in reversed(self.ops[e]):
                if not o.dma and o.fn is not None:
                    lasts.append(o)
                    break
            seen = set()
            for o in reversed(self.ops[e]):
                if o.dma and o.dsem not in seen:
                    seen.add(o.dsem)
                    lasts.append(o)
                    if len(seen) >= NDMASEM:
                        break
        for e in self.ENGS:
            o = Op(e, None, False)
            for d in lasts:
                if d.eng == e and not d.dma:
                    continue
                o.deps.append(d)
                d.signal = True
            self.ops[e].append(o)
        for b in self.bufs.values():
            b.w = None
            b.r = {}
            b.rd = []

    def emit(self):
        nc = self.nc
        for e in self.ENGS:
            c = 0
            for o in self.ops[e]:
                if o.dma:
                    continue
                if o.signal:
                    c += 1
                    o.semval = c
        with contextlib.ExitStack() as st:
            esem = {e: st.enter_context(nc.semaphore("s_" + e)) for e in self.ENGS}
            dsem = {}
            for e in self.ENGS:
                for i in range(min(NDMASEM, self.ndma[e])):
                    dsem[(e, i)] = st.enter_context(nc.semaphore("d_%s_%d" % (e, i)))
            block = st.enter_context(nc.Block())

            def run(ename, eng):
                waited = {}

                def wait(key, sem, val):
                    if waited.get(key, 0) >= val:
                        return
                    waited[key] = val
                    eng.wait_ge(sem, val)

                for o in self.ops[ename]:
                    if o.prewait is not None:
                        ds, v = o.prewait
                        wait(ds, dsem[ds], v)
                    for d in o.deps:
                        if d.dma:
                            wait(d.dsem, dsem[d.dsem], d.semval)
                        else:
                            wait(d.eng, esem[d.eng], d.semval)
                    if o.fn is None:
                        continue
                    ins = o.fn(eng)
                    if o.dma:
                        ins.then_inc(dsem[o.dsem], 16)
                    elif o.signal:
                        ins.then_inc(esem[ename], 1)

            @block.tensor
            def _(pe):
                run("pe", pe)

            @block.scalar
            def _(act):
                run("act", act)

            @block.vector
            def _(dve):
                run("dve", dve)

            @block.gpsimd
            def _(pool):
                run("pool", pool)

            @block.sync
            def _(sp):
                run("sp", sp)


def _bucket_table(nmax):
    n = np.arange(nmax, dtype=np.float64)
    nf = np.maximum(n, 16.0)
    large = 16 + np.floor(np.log(nf / 16.0) / np.log(8.0) * 16.0 + 1e-9).astype(np.int64)
    large = np.minimum(large, 31)
    return np.where(n < 16, n.astype(np.int64), large)


def _consts():
    i = np.arange(128)[:, None]
    c = np.arange(1024)[None, :]
    rel = c - 384 - i
    bt = _bucket_table(1024)
    bidx = bt[np.maximum(rel, 0)]
    mmask = np.where(rel < 0, 8.0 * NEG, 0.0).astype(np.float32)
    mpast = (rel > 0).astype(np.float32)
    negm = np.where(rel > 0, 0.0, NEG).astype(np.float32)
    ident = np.eye(128, dtype=np.float32)
    ustrict = (np.arange(128)[:, None] > np.arange(128)[None, :]).astype(np.float32)
    sel = np.zeros((16, 16, 128), np.float32)
    for w in range(16):
        sel[w, w, :] = 1.0
    sel = sel.reshape(16, 16 * 128)
    inv = np.zeros((16, 2, 8), np.float32)
    for t in range(16):
        inv[t, :, (t // 2):] = 1.0
    inv = np.broadcast_to(inv.reshape(1, 256), (128, 256)).copy()
    selrow = np.zeros((2, 2, 128), np.float32)
    selrow[0, 0, :] = 1.0
    selrow[1, 1, :] = 1.0
    selrow = selrow.reshape(2, 256)
    return dict(bidx=bidx, mmask=mmask, mpast=mpast, negm=negm, ident=ident, ustrict=ustrict,
                sel=sel, inv=inv, selrow=selrow)


def build(stop_after=None):
    nc = bass.Bass("TRN2", target_bir_lowering=False)

    def din(name, shape, dt=F32):
        return nc.dram_tensor(name, list(shape), dt, kind="ExternalInput").ap()

    x = din("x", [NSEQ, SEQ, D])
    cT = din("cT", [128, 8, 2])
    w_ada = din("w_ada", [D, 6 * D])
    b_adaT = din("b_adaT", [128, 48])
    b_ada_row = din("b_ada_row", [1, 2048])
    g_mixT = din("g_mixT", [128, 8])
    g_ffnT = din("g_ffnT", [128, 8])
    g_fin = din("g_fin", [1, D])
    w_in = din("w_in", [D, 5120])
    w_bra = din("w_bra", [512, D])
    w_brb = din("w_brb", [512, D])
    w_out = din("w_out", [D, D])
    w_up = din("w_up", [D, 2 * DFF])
    w_convT = din("w_convT", [128, 44, 3])
    b_convT = din("b_convT", [128, 44])
    w_down = din("w_down", [DFF, D])
    relG = din("relG", [8, 128, 1024])
    b31 = din("b31", [128, 8])
    k_mmask = din("k_mmask", [128, 1024])
    k_mpast = din("k_mpast", [128, 1024])
    k_negm = din("k_negm", [128, 1024])
    k_ident = din("k_ident", [128, 128])
    k_ustrict = din("k_ustrict", [128, 128])
    k_sel = din("k_sel", [16, 2048])
    k_inv = din("k_inv", [128, 256])
    k_selrow = din("k_selrow", [2, 256])
    out = nc.dram_tensor("out", [NSEQ, SEQ, D], F32, kind="ExternalOutput").ap()
    x1s = nc.dram_tensor("x1s", [NSEQ, SEQ, D], F32, kind="Internal").ap()
    dbg = {}
    if DEBUG:
        dbg["hT"] = nc.dram_tensor("dbg_hT", [128, 8 * SEQ], BF16, kind="ExternalOutput").ap()
        dbg["oT"] = nc.dram_tensor("dbg_oT", [128, 8 * SEQ], BF16, kind="ExternalOutput").ap()
        dbg["combT"] = nc.dram_tensor("dbg_combT", [128, 8 * SEQ], BF16, kind="ExternalOutput").ap()
        dbg["h2T"] = nc.dram_tensor("dbg_h2T", [128, 8 * SEQ], BF16, kind="ExternalOutput").ap()
        dbg["yT"] = nc.dram_tensor("dbg_yT", [128, 22 * SEQ], BF16, kind="ExternalOutput").ap()
        dbg["misc"] = nc.dram_tensor("dbg_misc", [128, 1024], F32, kind="ExternalOutput").ap()

    K = 1024
    TOTAL = 207 * K
    with contextlib.ExitStack() as st:
        big = st.enter_context(nc.sbuf_tensor("big", [128, TOTAL // 4], F32))
        PS = [st.enter_context(nc.psum_tensor("ps%d" % i, [128, 512], F32)) for i in range(8)]
        S = Sched(nc)

        def VW(off, nbytes, dt=F32, parts=128):
            assert off % 4 == 0 and nbytes % 4 == 0 and off + nbytes <= TOTAL, (off, nbytes)
            v = big[0:parts, off // 4:(off + nbytes) // 4]
            if dt == BF16:
                v = v.bitcast(BF16)
            return v

        def psb(i):
            return PS[i][:, :].bitcast(BF16)

        A_OFF = 0
        B_OFF = 32 * K
        C_OFF = 64 * K
        Y_OFF = 32 * K
        D_OFF = 120 * K
        D_SZ = 71 * K
        CO = 191 * K

        hT = VW(A_OFF, 32 * K, BF16).rearrange("p (k n) -> p k n", k=8)
        oT = VW(B_OFF, 32 * K, BF16).rearrange("p (k n) -> p k n", k=8)
        combT = VW(C_OFF, 32 * K, BF16).rearrange("p (k n) -> p k n", k=8)
        Bh = VW(C_OFF, 16 * K, BF16).rearrange("p (h n) -> p h n", h=8)
        yT = VW(Y_OFF, 88 * K, BF16).rearrange("p (k n) -> p k n", k=22)

        co = [CO]

        def calloc(nbytes, dt=F32, parts=128):
            v = VW(co[0], nbytes, dt, parts)
            co[0] += (nbytes + 31) // 32 * 32
            assert co[0] <= TOTAL
            return v

        identb = calloc(256, BF16)
        ustrict = calloc(256, BF16)
        onesb = calloc(256, BF16)
        onesf = calloc(512, F32)
        selb = calloc(4096, BF16)
        selrow = calloc(1024, F32)
        mpast = calloc(2048, BF16)
        negm = calloc(2048, BF16)
        inv = calloc(1024, F32)
        neginv = calloc(1024, F32)
        b31s = calloc(32, F32)
        modT = calloc(384, F32).rearrange("p (c b) -> p c b", b=2)
        A1 = calloc(64, F32).rearrange("p (c b) -> p c b", b=2)
        A2 = calloc(64, F32).rearrange("p (c b) -> p c b", b=2)
        gmixT = calloc(32, F32)
        gffnT = calloc(32, F32)
        wconv = calloc(44 * 3 * 4, F32).rearrange("p (c i) -> p c i", i=3)
        bconv = calloc(44 * 4, F32)
        badaT = calloc(48 * 4, F32)
        small = calloc(256, F32)
        CONST_END = co[0]

        def dma(eng, out_, in_, reads=(), writes=()):
            return S.op(eng, lambda e: e.dma_start(out=out_, in_=in_), reads=reads, writes=writes, dma=True)

        def mm(o, l, r, start, stop, reads, writes):
            return S.op("pe", lambda e: e.matmul(o, lhsT=l, rhs=r, start=start, stop=stop), reads=reads, writes=writes)

        def act(o, i, func, reads, writes, bias=None, scale=None, accum=None):
            kw = {}
            if bias is not None:
                kw["bias"] = bias
            if scale is not None:
                kw["scale"] = scale
            if accum is not None:
                kw["accum_out"] = accum
            return S.op("act", lambda e: e.activation(out=o, in_=i, func=func, **kw), reads=reads, writes=writes)

        def tsc(eng, o, i, s1, s2, op0, op1, reads, writes):
            if s2 is None:
                return S.op(eng, lambda e: e.tensor_scalar(out=o, in0=i, scalar1=s1, scalar2=None, op0=op0), reads=reads, writes=writes)
            return S.op(eng, lambda e: e.tensor_scalar(out=o, in0=i, scalar1=s1, scalar2=s2, op0=op0, op1=op1), reads=reads, writes=writes)

        def tt(eng, o, a, b, op, reads, writes):
            return S.op(eng, lambda e: e.tensor_tensor(out=o, in0=a, in1=b, op=op), reads=reads, writes=writes)

        def stt(o, a, s, b, op0, op1, reads, writes, accum=None):
            if accum is None:
                return S.op("dve", lambda e: e.scalar_tensor_tensor(out=o, in0=a, scalar=s, in1=b, op0=op0, op1=op1), reads=reads, writes=writes)
            return S.op("dve", lambda e: e.scalar_tensor_tensor(out=o, in0=a, scalar=s, in1=b, op0=op0, op1=op1, accum_out=accum), reads=reads, writes=writes)

        def cp(eng, o, i, reads, writes):
            if eng == "act":
                return S.op("act", lambda e: e.activation(out=o, in_=i, func=AF.Identity), reads=reads, writes=writes)
            return S.op(eng, lambda e: e.tensor_copy(out=o, in_=i), reads=reads, writes=writes)

        stg = VW(D_OFF, 4096, F32)
        stg2 = VW(D_OFF + 4096, 4096, F32)
        dma("sp", stg[:, 0:128], k_ident, writes=["stg"])
        cp("dve", identb, stg[:, 0:128], ["stg"], ["identb"])
        dma("sp", stg[:, 128:256], k_ustrict, writes=["stgb"])
        cp("dve", ustrict, stg[:, 128:256], ["stgb"], ["ustrict"])
        S.op("pool", lambda e: e.memset(onesb, 1.0), writes=["onesb"])
        S.op("pool", lambda e: e.memset(onesf, 1.0), writes=["onesf"])
        dma("pool", selb[0:16, :], k_sel, writes=["selb"])
        dma("sp", selrow[0:2, :], k_selrow, writes=["selrow"])
        dma("pool", mpast, k_mpast, writes=["mpast"])
        dma("pool", negm, k_negm, writes=["negm"])
        dma("sp", inv, k_inv, writes=["inv"])
        tsc("dve", neginv, inv, -1e30, None, ALU.mult, None, ["inv"], ["neginv"])
        dma("sp", b31s, b31, writes=["b31s"])
        dma("sp", gmixT, g_mixT, writes=["gmixT"])
        dma("sp", gffnT, g_ffnT, writes=["gffnT"])
        dma("sp", wconv, w_convT, writes=["wconv"])
        dma("sp", bconv, b_convT, writes=["bconv"])
        dma("sp", badaT, b_adaT, writes=["badaT"])

        cTt = VW(D_OFF + 8192, 64, F32).rearrange("p (k b) -> p k b", b=2)
        cactb = VW(D_OFF + 8192 + 64, 32, BF16).rearrange("p (k b) -> p k b", b=2)
        cactf = VW(D_OFF + 8192 + 128, 64, F32).rearrange("p (k b) -> p k b", b=2)
        dma("sp", cTt, cT, writes=["cTt"])
        act(cactb, cTt, AF.Silu, ["cTt"], ["cactb"])
        act(cactf, cTt, AF.Silu, ["cTt"], ["cactf"])
        gtrow = VW(D_OFF + 8192 + 256, 8192, F32, parts=2)
        badar = VW(D_OFF + 8192 + 256 + 8192, 8192, F32, parts=2)
        dma("sp", badar[0:1, :], b_ada_row, writes=["badar0"])
        dma("sp", badar[1:2, :], b_ada_row, writes=["badar1"])
        WB_OFF = D_OFF + 32 * K
        wada_v = w_ada.rearrange("(k p) n -> p k n", p=128)
        for blk in range(8):
            wb = VW(WB_OFF + (blk % 2) * 12 * K, 12 * K, BF16).rearrange("p (k n) -> p k n", k=8)
            wn = "wada%d" % (blk % 2)
            dma("pool", wb, wada_v[:, :, blk * 768:(blk + 1) * 768], writes=[wn])
            for j in range(6):
                cidx = blk * 6 + j
                for k in range(8):
                    mm(PS[0][:, 2 * cidx:2 * cidx + 2], wb[:, k, j * 128:(j + 1) * 128], cactb[:, k, :], k == 0, k == 7,
                       [wn, "cactb"], ["ps0"])
        tt("dve", modT, PS[0][:, 0:96].rearrange("p (c b) -> p c b", b=2),
           badaT.rearrange("p (c o) -> p c o", o=1).to_broadcast([128, 48, 2]), ALU.add, ["ps0", "badaT"], ["modT"])
        tsc("dve", A1, modT[:, 8:16, :], 1.0, None, ALU.add, None, ["modT"], ["A1"])
        tt("dve", A1, A1, gmixT.rearrange("p (c o) -> p c o", o=1).to_broadcast([128, 8, 2]), ALU.mult, ["A1", "gmixT"], ["A1"])
        tsc("dve", A2, modT[:, 32:40, :], 1.0, None, ALU.add, None, ["modT"], ["A2"])
        tt("dve", A2, A2, gffnT.rearrange("p (c o) -> p c o", o=1).to_broadcast([128, 8, 2]), ALU.mult, ["A2", "gffnT"], ["A2"])
        GT_OFF = D_OFF + 32 * K + 24 * K
        S.barrier()
        wf = VW(D_OFF + 26 * K, 32 * K, F32).rearrange("p (k n) -> p k n", k=8)
        for g, c0 in enumerate((2048, 5120)):
            dma("sp", wf, wada_v[:, :, c0:c0 + 1024], writes=["wf"])
            for hh in range(2):
                for k in range(8):
                    mm(PS[1][0:2, :], cactf[:, k, :], wf[:, k, hh * 512:(hh + 1) * 512], k == 0, k == 7, ["wf", "cactf"], ["ps1"])
                tt("dve", gtrow[0:2, g * 1024 + hh * 512:g * 1024 + (hh + 1) * 512], PS[1][0:2, :],
                   badar[0:2, g * 1024 + hh * 512:g * 1024 + (hh + 1) * 512], ALU.add, ["ps1", "badar0", "badar1"], ["gtrow"])
        GTB = D_OFF + D_SZ - 16 * K

        def GT(s, g):
            return VW(GTB + (s * 2 + g) * 4096, 4096, F32)

        for s in range(NSEQ):
            for g in range(2):
                for hh in range(2):
                    mm(PS[2][:, :], selrow[0:2, s * 128:(s + 1) * 128], gtrow[0:2, g * 1024 + hh * 512:g * 1024 + (hh + 1) * 512],
                       True, True, ["selrow", "gtrow"], ["ps2"])
                    cp("act", GT(s, g)[:, hh * 512:(hh + 1) * 512], PS[2][:, :], ["ps2"], ["GT"])
        S.barrier()
        STOP = [False]

        def stop(tag):
            if stop_after == tag:
                STOP[0] = True
            return STOP[0]
        DW = GTB - D_OFF

        def rms_tile(xt_ap, xtn, ssn, tag):
            junk = VW(D_OFF + 0, 4096, F32)
            ss = small[:, 0:1]
            lnv = small[:, 1:2]
            rstd = small[:, 2:3]
            stt(junk, xt_ap, 1.0, xt_ap, ALU.mult, ALU.mult, [xtn], ["junk", "ss"], accum=ss)
            act(lnv, ss, AF.Ln, ["ss"], ["lnv"], bias=1e-6, scale=1.0 / 1024.0)
            act(rstd, lnv, AF.Exp, ["lnv"], ["rstd"], scale=-0.5)
            return rstd

        def norm_to_T(xt_ap, xtn, dstT, Amod, Bmod_c0, s, t, xnb, xnbn, psi):
            rstd = rms_tile(xt_ap, xtn, None, None)
            tsc("dve", xnb, xt_ap, rstd, None, ALU.mult, None, [xtn, "rstd"], [xnbn])
            pn = "ps%d" % psi
            pv = psb(psi)
            for c in range(8):
                S.op("pe", lambda e, c=c: e.transpose(out=pv[:, c * 128:(c + 1) * 128], in_=xnb[:, c * 128:(c + 1) * 128], identity=identb),
                     reads=[xnbn, "identb"], writes=[pn])
            for c in range(8):
                o = dstT[:, c, t * 128:(t + 1) * 128]
                i = pv[:, c * 128:(c + 1) * 128]
                if t % 2 == 0:
                    act(o, i, AF.Identity, [pn, "A1", "A2", "modT"], ["dstT"], bias=modT[:, Bmod_c0 + c, s:s + 1], scale=Amod[:, c, s:s + 1])
                else:
                    tsc("dve", o, i, Amod[:, c, s:s + 1], modT[:, Bmod_c0 + c, s:s + 1], ALU.mult, ALU.add, [pn, "A1", "A2", "modT"], ["dstT"])

        w_in_v = w_in.rearrange("(k p) n -> p k n", p=128)

        for s in range(NSEQ):
            if stop("setup"):
                break
            for t in range(16):
                xt = VW(D_OFF + 4096 + (t % 2) * 4096, 4096, F32)
                xtn = "xt%d" % (t % 2)
                dma("sp", xt, x[s, t * 128:(t + 1) * 128, :], writes=[xtn])
                xnb = VW(D_OFF + 12288 + (t % 2) * 2048, 2048, BF16)
                norm_to_T(xt, xtn, hT, A1, 0, s, t, xnb, "xnb%d" % (t % 2), 6 + (t % 2))
            S.barrier()
            if DEBUG and s == 0:
                dma("sp", dbg["hT"], VW(A_OFF, 32 * K, BF16), reads=["dstT"])
                S.barrier()

            if stop("p1"):
                break
            mm_f = VW(D_OFF, 4096, F32)
            dma("sp", mm_f, k_mmask, writes=["mm_f"])
            for h in range(8):
                gtile = VW(D_OFF + 4096 + (h % 2) * 4096, 4096, F32)
                gn = "gt%d" % (h % 2)
                dma("sp", gtile, relG[h], writes=[gn])
                tsc("dve", gtile, gtile, b31s[:, h:h + 1], 8.0, ALU.subtract, ALU.mult, [gn, "b31s"], [gn])
                tt("dve", Bh[:, h, :], gtile, mm_f, ALU.add, [gn, "mm_f"], ["Bh"])
            S.barrier()
            o_ = [D_OFF]

            def dalloc(nbytes, dt=F32, parts=128):
                v = VW(o_[0], nbytes, dt, parts)
                o_[0] += (nbytes + 31) // 32 * 32
                assert o_[0] <= GTB, o_[0] - GTB
                return v

            wq = [dalloc(2048, BF16).rearrange("p (k n) -> p k n", k=8) for _ in range(1)]
            wk = [dalloc(2048, BF16).rearrange("p (k n) -> p k n", k=8) for _ in range(1)]
            wv = [dalloc(2048, BF16).rearrange("p (k n) -> p k n", k=8) for _ in range(1)]
            qT = dalloc(4096, BF16)
            kT = dalloc(4096, BF16)
            Vaug = dalloc(8192, BF16).rearrange("p (t n) -> p t n", t=16)
            negT = dalloc(4096, BF16)
            gm = dalloc(1024, F32)
            top8 = dalloc(1024, F32)
            mge = dalloc(1024, F32)
            negq = dalloc(512, BF16)
            kms = dalloc(32, F32)
            kmh = dalloc(16, BF16)
            kmr = dalloc(32, F32)
            kml = dalloc(16, BF16)
            PT = [dalloc(1024, BF16) for _ in range(3)]
            e_t = [dalloc(2048, F32) for _ in range(2)]
            sp_t = [dalloc(2048, F32) for _ in range(2)]
            lkm = [dalloc(1024, BF16) for _ in range(2)]
            ssum = [dalloc(1024, BF16) for _ in range(2)]
            t_t = [dalloc(2048, F32) for _ in range(2)]
            a_t = [dalloc(1024, BF16) for _ in range(2)]
            rr = e_t[0]
            lnr = e_t[1]
            bcs = sp_t[0]

            S.op("pool", lambda e: e.memset(Vaug, 0.0), writes=["Vaug"])
            S.op("pool", lambda e: e.memset(Vaug[:, :, 64:65], 1.0), reads=[], writes=["Vaug"])
            S.op("pool", lambda e: e.memset(Vaug[:, :, 128:129], 1.0), reads=[], writes=["Vaug"])

            zc = [0]

            def zbank():
                i = zc[0] % 3
                zc[0] += 1
                return i

            def project_pair(kind, p):
                base = 0 if kind == 0 else 1536
                pp = 0
                dma("pool", wq[pp], w_in_v[:, :, base + 128 * p:base + 128 * p + 128], writes=["wq%d" % pp])
                dma("pool", wk[pp], w_in_v[:, :, base + 512 + 128 * p:base + 512 + 128 * p + 128], writes=["wk%d" % pp])
                dma("pool", wv[pp], w_in_v[:, :, base + 1024 + 128 * p:base + 1024 + 128 * p + 128], writes=["wv%d" % pp])
                for (wt, wn, dst, dn) in ((wq[pp], "wq%d" % pp, qT, "qT"), (wk[pp], "wk%d" % pp, kT, "kT")):
                    for tq in range(4):
                        zi = zbank()
                        for k in range(8):
                            mm(PS[zi][:, :], wt[:, k, :], hT[:, k, tq * 512:(tq + 1) * 512], k == 0, k == 7, [wn, "hT"], ["ps%d" % zi])
                        cp("act" if tq % 2 == 0 else "dve", dst[:, tq * 512:(tq + 1) * 512], PS[zi][:, :], ["ps%d" % zi], [dn])
                for g in range(4):
                    zi = zbank()
                    for tl in range(4):
                        t = g * 4 + tl
                        for k in range(8):
                            mm(PS[zi][:, tl * 128:(tl + 1) * 128], hT[:, k, t * 128:(t + 1) * 128], wv[pp][:, k, :], k == 0, k == 7,
                               ["wv%d" % pp, "hT"], ["ps%d" % zi])
                    pv3 = PS[zi][:, :].rearrange("p (t n) -> p t n", t=4)
                    cp("act", Vaug[:, g * 4:(g + 1) * 4, 0:64], pv3[:, :, 0:64], ["ps%d" % zi], ["Vaug"])
                    cp("dve", Vaug[:, g * 4:(g + 1) * 4, 192:256], pv3[:, :, 64:128], ["ps%d" % zi], ["Vaug"])

            def moba_gate():
                tensor_reduce = lambda e: e.tensor_reduce(out=kms, in_=kT.rearrange("p (n j) -> p n j", j=256), axis=AX.X, op=ALU.add)
                S.op("dve", tensor_reduce, reads=["kT"], writes=["kms"])
                tsc("dve", kms, kms, 1.0 / 256.0, None, ALU.mult, None, ["kms"], ["kms"])
                cp("dve", kmh, kms, ["kms"], ["kmh"])
                tt("dve", kmr, kms, kmh, ALU.subtract, ["kms", "kmh"], ["kmr"])
                cp("dve", kml, kmr, ["kmr"], ["kml"])
                gm4 = gm.rearrange("p (t h n) -> p t h n", t=16, h=2)
                ni4 = neginv.rearrange("p (t h n) -> p t h n", t=16, h=2)
                for hd in range(2):
                    hs = slice(64 * hd, 64 * hd + 64)
                    gb = 3 + hd
                    gv = PS[gb][:, 0:128].rearrange("p (t n) -> p t n", t=16)
                    for t in range(16):
                        mm(gv[:, t, :], qT[hs, t * 128:(t + 1) * 128], kmh[hs, :], True, False, ["qT", "kmh"], ["ps%d" % gb])
                        mm(gv[:, t, :], qT[hs, t * 128:(t + 1) * 128], kml[hs, :], False, True, ["qT", "kml"], ["ps%d" % gb])
                    tt("dve", gm4[:, :, hd, :], gv, ni4[:, :, hd, :], ALU.add, ["ps%d" % gb, "neginv"], ["gm"])
                gm3 = gm.rearrange("p (g n) -> p g n", n=8)
                t83 = top8.rearrange("p (g n) -> p g n", n=8)
                for g in range(32):
                    S.op("dve", lambda e, g=g: e.max(out=t83[:, g, :], in_=gm3[:, g, :]), reads=["gm"], writes=["top8"])
                tt("dve", mge.rearrange("p (g n) -> p g n", n=8), gm3, t83[:, :, 2:3].to_broadcast([128, 32, 8]), ALU.is_ge,
                   ["gm", "top8"], ["mge"])
                tt("dve", mge, mge, inv, ALU.max, ["mge", "inv"], ["mge"])
                tsc("dve", negq, mge, -8.0 * NEG, 8.0 * NEG, ALU.mult, ALU.add, ["mge"], ["negq"])
                for half in range(2):
                    pv = psb(4)
                    for tl in range(8):
                        t = half * 8 + tl
                        S.op("pe", lambda e, t=t, tl=tl, pv=pv: e.transpose(out=pv[0:16, tl * 128:(tl + 1) * 128], in_=negq[:, t * 16:(t + 1) * 16], identity=identb),
                             reads=["negq", "identb"], writes=["ps4"])
                    cp("dve", negT[0:16, half * 1024:(half + 1) * 1024], pv[0:16, :], ["ps4"], ["negT"])

            acc_c = [0]

            def moba_head(p, hd):
                head = 2 * p + hd
                hs = slice(64 * hd, 64 * hd + 64)
                for qt in range(4):
                    ai = 5 + (acc_c[0] % 2)
                    acc_c[0] += 1
                    an = "ps%d" % ai
                    nch = 4 * qt + 4
                    qs = slice(qt * 512, (qt + 1) * 512)
                    for kc in range(nch):
                        zi = zbank()
                        zn = "ps%d" % zi
                        n = kc // 2
                        d0 = 512 * qt - 128 * kc
                        use_sel = n <= 2 * qt
                        use_toe = d0 < 240
                        mm(PS[zi][:, :], kT[hs, kc * 128:(kc + 1) * 128], qT[hs, qs], True, not (use_sel or use_toe), ["kT", "qT"], [zn])
                        if use_sel:
                            w = hd * 8 + n
                            mm(PS[zi][:, :], selb[0:16, w * 128:(w + 1) * 128], negT[0:16, qs], False, not use_toe, ["selb", "negT"], [zn])
                        if use_toe:
                            c0 = d0 + 384
                            mm(PS[zi][:, :], identb, Bh[:, head, c0:c0 + 512], False, True, ["identb", "Bh"], [zn])
                        pi = kc % 3
                        act(PT[pi], PS[zi][:, :], AF.Exp, [zn, "b31s"], ["PT%d" % pi], bias=b31s[:, head:head + 1], scale=0.125)
                        if hd == 0:
                            mm(PS[ai][0:65, :], Vaug[:, kc, 0:65], PT[pi], kc == 0, kc == nch - 1, ["Vaug", "PT%d" % pi], [an])
                        else:
                            mm(PS[ai][:, :], Vaug[:, kc, 128:256], PT[pi], kc == 0, kc == nch - 1, ["Vaug", "PT%d" % pi], [an])
                    srow = 64 if hd == 0 else 0
                    act(lnr[0:1, 0:512], PS[ai][srow:srow + 1, :], AF.Ln, [an], ["e1"])
                    act(rr[0:1, 0:512], lnr[0:1, 0:512], AF.Exp, ["e1"], ["e0"], scale=-1.0)
                    mm(PS[7][:, :], onesf[0:1, :], rr[0:1, 0:512], True, True, ["onesf", "e0"], ["ps7"])
                    cp("act", bcs[hs, 0:512], PS[7][hs, :], ["ps7"], ["sp0"])
                    tt("dve", oT[hs, p, qs], PS[ai][hs, :], bcs[hs, 0:512], ALU.mult, [an, "sp0"], ["oT"])

            c2 = [C_OFF + 16 * K]

            def c2alloc(nbytes, dt=F32):
                v = VW(c2[0], nbytes, dt)
                c2[0] += nbytes
                assert c2[0] <= C_OFF + 32 * K
                return v

            SBS = [dict(e=e_t, sp=sp_t, lkm=lkm, ssum=ssum, t=t_t, a=a_t, sfx="", L=3, acc=5),
                   dict(e=[c2alloc(2048) for _ in range(2)], sp=[c2alloc(2048) for _ in range(2)],
                        lkm=[c2alloc(1024, BF16) for _ in range(2)], ssum=[c2alloc(1024, BF16) for _ in range(2)],
                        t=[c2alloc(2048) for _ in range(2)], a=[dalloc(1024, BF16) for _ in range(2)], sfx="b", L=4, acc=6)]

            accrot = [0]

            def sb_chunk(p, hd, qt, idx, ai):
                B = SBS[hd]
                sx = B["sfx"]
                hs = slice(64 * hd, 64 * hd + 64)
                li = B["L"]
                ln_ = "ps%d" % li
                an = "ps%d" % ai
                nch = 4 * qt + 4
                qs = slice(qt * 512, (qt + 1) * 512)
                kc = nch - 1 - idx
                b2 = idx % 2
                d0 = 512 * qt - 128 * kc
                diag = kc >= 4 * qt
                c0 = d0 + 384
                en, spn, lkn, tn, an_ = ("e%d%s" % (b2, sx), "sp%d%s" % (b2, sx), "lkm%d%s" % (b2, sx), "t%d%s" % (b2, sx), "a%d%s" % (b2, sx))
                e_b, sp_b, lk_b, t_b, a_b = B["e"][b2], B["sp"][b2], B["lkm"][b2], B["t"][b2], B["a"][b2]
                zi = zbank()
                zn = "ps%d" % zi
                mm(PS[zi][:, :], kT[hs, kc * 128:(kc + 1) * 128], qT[hs, qs], True, True, ["kT", "qT"], [zn])
                act(e_b, PS[zi][:, :], AF.Exp, [zn], [en], scale=-0.125)
                act(sp_b, e_b, AF.Ln, [en], [spn], bias=1.0)
                stt(lk_b, PS[zi][:, :], 0.125, sp_b, ALU.mult, ALU.add, [zn, spn], [lkn])
                if diag:
                    tt("pool", lk_b, lk_b, mpast[:, c0:c0 + 512], ALU.mult, [lkn, "mpast"], [lkn])
                yield
                mm(PS[li][:, :], ustrict, lk_b, True, idx == 0, ["ustrict", lkn], [ln_])
                if idx > 0:
                    mm(PS[li][:, :], onesb, B["ssum"][(idx - 1) % 2], False, True, ["onesb", "ssum%d%s" % ((idx - 1) % 2, sx)], [ln_])
                if kc > 0:
                    if idx == 0:
                        cp("pool", B["ssum"][0], lk_b, [lkn], ["ssum0%s" % sx])
                    else:
                        tt("pool", B["ssum"][idx % 2], B["ssum"][(idx - 1) % 2], lk_b, ALU.add,
                           ["ssum%d%s" % ((idx - 1) % 2, sx), lkn], ["ssum%d%s" % (idx % 2, sx)])
                stt(t_b, PS[li][:, :], -1.0, sp_b, ALU.mult, ALU.subtract, [ln_, spn], [tn])
                if diag:
                    tt("pool", t_b, t_b, negm[:, c0:c0 + 512], ALU.add, [tn, "negm"], [tn])
                yield
                act(a_b, t_b, AF.Exp, [tn], [an_])
                if hd == 0:
                    mm(PS[ai][0:64, :], Vaug[:, kc, 0:64], a_b, idx == 0, kc == 0, ["Vaug", an_], [an])
                else:
                    mm(PS[ai][:, :], Vaug[:, kc, 128:256], a_b, idx == 0, kc == 0, ["Vaug", an_], [an])
                if kc == 0:
                    cp("act", oT[hs, 4 + p, qs], PS[ai][hs, :], [an], ["oT"])

            def sb_stream(p, hd):
                for qt in range(4):
                    ai = 5 + (accrot[0] % 3)
                    accrot[0] += 1
                    for idx in range(4 * qt + 4):
                        yield sb_chunk(p, hd, qt, idx, ai)

            def sb_pair(p):
                streams = [sb_stream(p, 0), sb_stream(p, 1)]
                active = []
                more = True
                while more or active:
                    for g in list(active):
                        try:
                            next(g)
                        except StopIteration:
                            active.remove(g)
                    more = False
                    for st_ in streams:
                        g = next(st_, None)
                        if g is not None:
                            more = True
                            next(g)
                            active.append(g)

            for kind in range(2):
                for p in range(4):
                    project_pair(kind, p)
                    if kind == 0:
                        moba_gate()
                        for hd in range(2):
                            moba_head(p, hd)
                    else:
                        sb_pair(p)
            S.barrier()
            if DEBUG and s == 0:
                dma("sp", dbg["oT"], VW(B_OFF, 32 * K, BF16), reads=["oT"])
                S.barrier()

            if stop("p2"):
                break
            o_[0] = D_OFF
            wbra = dalloc(8192, BF16).rearrange("p (k n) -> p k n", k=4)
            wbrb = dalloc(8192, BF16).rearrange("p (k n) -> p k n", k=4)
            wg = dalloc(16 * K, BF16).rearrange("p (k n) -> p k n", k=8)
            sg = [dalloc(2048, F32) for _ in range(2)]
            t1 = [dalloc(2048, F32) for _ in range(2)]
            t2 = [dalloc(2048, F32) for _ in range(2)]
            dma("pool", wbra, w_bra.rearrange("(k p) n -> p k n", p=128), writes=["wbra"])
            dma("pool", wbrb, w_brb.rearrange("(k p) n -> p k n", p=128), writes=["wbrb"])
            cnt = 0
            for cg in range(2):
                for j in range(4):
                    c = cg * 4 + j
                    dma("pool", wg[:, :, j * 256:j * 256 + 128], w_in_v[:, :, 3072 + 128 * c:3072 + 128 * c + 128], reads=[], writes=["wga%d" % j])
                    dma("pool", wg[:, :, j * 256 + 128:j * 256 + 256], w_in_v[:, :, 4096 + 128 * c:4096 + 128 * c + 128], reads=[], writes=["wgb%d" % j])
                for j in range(4):
                    c = cg * 4 + j
                    for tq in range(4):
                        ts_ = slice(tq * 512, (tq + 1) * 512)
                        b2 = cnt % 2
                        cnt += 1
                        pa, pb, pga, pgb = (0, 1, 2, 3) if b2 == 0 else (4, 5, 6, 7)
                        for k in range(4):
                            mm(PS[pa][:, :], wbra[:, k, c * 128:(c + 1) * 128], oT[:, k, ts_], k == 0, k == 3, ["wbra", "oT"], ["ps%d" % pa])
                        for k in range(4):
                            mm(PS[pb][:, :], wbrb[:, k, c * 128:(c + 1) * 128], oT[:, 4 + k, ts_], k == 0, k == 3, ["wbrb", "oT"], ["ps%d" % pb])
                        for k in range(8):
                            mm(PS[pga][:, :], wg[:, k, j * 256:j * 256 + 128], hT[:, k, ts_], k == 0, k == 7, ["wga%d" % j, "hT"], ["ps%d" % pga])
                        for k in range(8):
                            mm(PS[pgb][:, :], wg[:, k, j * 256 + 128:j * 256 + 256], hT[:, k, ts_], k == 0, k == 7, ["wgb%d" % j, "hT"], ["ps%d" % pgb])
                        act(sg[0], PS[pga][:, :], AF.Sigmoid, ["ps%d" % pga], ["sg0"])
                        act(sg[1], PS[pgb][:, :], AF.Sigmoid, ["ps%d" % pgb], ["sg1"])
                        tt("dve", t1[b2], PS[pa][:, :], sg[0], ALU.mult, ["ps%d" % pa, "sg0"], ["t1%d" % b2])
                        tt("dve", t2[b2], PS[pb][:, :], sg[1], ALU.mult, ["ps%d" % pb, "sg1"], ["t2%d" % b2])
                        tt("pool", combT[:, c, ts_], t1[b2], t2[b2], ALU.add, ["t1%d" % b2, "t2%d" % b2], ["combT"])
            S.barrier()
            if DEBUG and s == 0:
                dma("sp", dbg["combT"], VW(C_OFF, 32 * K, BF16), reads=["combT"])
                S.barrier()

            if stop("p3a"):
                break
            o_[0] = D_OFF + 4096
            wo = dalloc(16 * K, BF16).rearrange("p (k n) -> p k n", k=8)
            xts = [dalloc(4096, F32) for _ in range(2)]
            x1t = [dalloc(4096, F32) for _ in range(2)]
            xnbs = [dalloc(2048, BF16) for _ in range(2)]
            dma("pool", wo, w_out.rearrange("(k p) n -> p k n", p=128), writes=["wo"])
            for t in range(16):
                b2 = t % 2
                tks = slice(t * 128, (t + 1) * 128)
                dma("sp", xts[b2], x[s, tks, :], writes=["xts%d" % b2])
                for hh in range(2):
                    pi = 2 * b2 + hh
                    for k in range(8):
                        mm(PS[pi][:, :], combT[:, k, tks], wo[:, k, hh * 512:(hh + 1) * 512], k == 0, k == 7, ["combT", "wo"], ["ps%d" % pi])
                    tt("dve", x1t[b2][:, hh * 512:(hh + 1) * 512], PS[pi][:, :], GT(s, 0)[:, hh * 512:(hh + 1) * 512], ALU.mult,
                       ["ps%d" % pi, "GT"], ["x1t%d" % b2])
                import os as _os
                if not _os.environ.get("SKIP_ADD"):
                    tt("pool", x1t[b2], x1t[b2], xts[b2], ALU.add, ["x1t%d" % b2, "xts%d" % b2], ["x1t%d" % b2])
                if not _os.environ.get("SKIP_STORE"):
                    dma("sp", x1s[s, tks, :], x1t[b2], reads=["x1t%d" % b2], writes=["x1s"])
                if not _os.environ.get("SKIP_NORM"):
                    norm_to_T(x1t[b2], "x1t%d" % b2, hT, A2, 24, s, t, xnbs[b2], "xnbs%d" % b2, 6 + b2)
            S.barrier()
            if DEBUG and s == 0:
                dma("sp", dbg["h2T"], VW(A_OFF, 32 * K, BF16), reads=["dstT"])
                S.barrier()

            if stop("p3b"):
                break
            o_[0] = D_OFF
            wu = [dalloc(4096, BF16).rearrange("p (k n) -> p k n", k=8) for _ in range(2)]
            ub3 = [dalloc(8224, F32) for _ in range(3)]
            cvb = [dalloc(8192, F32) for _ in range(2)]
            w_up_v = w_up.rearrange("(k p) n -> p k n", p=128)
            for st_ in range(3):
                S.op("pool", lambda e, a=ub3[st_]: e.memset(a[:, 0:8], 0.0), writes=["ub%d" % st_])
            for m in range(22):
                b2 = m % 2
                dma("pool", wu[b2][:, :, 0:128], w_up_v[:, :, 128 * m:128 * m + 128], writes=["wu%d" % b2])
                dma("pool", wu[b2][:, :, 128:256], w_up_v[:, :, DFF + 128 * m:DFF + 128 * m + 128], reads=[], writes=["wu%da" % b2])
                for vg in range(2):
                    ui = (2 * m + vg) % 3
                    un = "ub%d" % ui
                    for tq in range(4):
                        zi = zbank()
                        for k in range(8):
                            mm(PS[zi][:, :], wu[b2][:, k, vg * 128:(vg + 1) * 128], hT[:, k, tq * 512:(tq + 1) * 512], k == 0, k == 7,
                               ["wu%d" % b2, "wu%da" % b2, "hT"], ["ps%d" % zi])
                        cp("act", ub3[ui][:, 8 + tq * 512:8 + (tq + 1) * 512], PS[zi][:, :], ["ps%d" % zi], [un])
                    ch = m + 22 * vg
                    u = ub3[ui]
                    cv = cvb[vg]
                    cn = "cv%d" % vg
                    tsc("pool", cv, u[:, 8:8 + 2048], wconv[:, ch, 2:3], bconv[:, ch:ch + 1], ALU.mult, ALU.add, [un, "wconv", "bconv"], [cn])
                    stt(cv, u[:, 7:7 + 2048], wconv[:, ch, 1:2], cv, ALU.mult, ALU.add, [un, cn, "wconv"], [cn])
                    stt(cv, u[:, 6:6 + 2048], wconv[:, ch, 0:1], cv, ALU.mult, ALU.add, [un, cn, "wconv"], [cn])
                act(cvb[1], cvb[1], AF.Gelu_apprx_tanh, ["cv1"], ["cv1"])
                tt("dve", yT[:, m, :], cvb[1], cvb[0], ALU.mult, ["cv0", "cv1"], ["yT"])
            S.barrier()
            if DEBUG and s == 0:
                dma("sp", dbg["yT"], VW(Y_OFF, 88 * K, BF16), reads=["yT"])
                S.barrier()

            if stop("p4"):
                break
            o_[0] = D_OFF + 4096
            wd = dalloc(44 * K, BF16).rearrange("p (k n) -> p k n", k=22)
            gfin = VW(A_OFF + 16384, 4096, F32)
            xq = [VW(A_OFF + 20480 + i * 4096, 4096, F32) for i in range(2)]
            x2t = [VW(A_OFF + i * 4096, 4096, F32) for i in range(2)]
            ot = [VW(A_OFF + 8192 + i * 4096, 4096, F32) for i in range(2)]
            dma("pool", wd, w_down.rearrange("(k p) n -> p k n", p=128), writes=["wd"])
            dma("sp", gfin, g_fin[0].partition_broadcast(128), writes=["gfin"])
            for t in range(16):
                b2 = t % 2
                tks = slice(t * 128, (t + 1) * 128)
                dma("sp", xq[b2], x1s[s, tks, :], reads=["x1s"], writes=["xq%d" % b2])
                for hh in range(2):
                    pi = 2 * b2 + hh
                    for k in range(22):
                        mm(PS[pi][:, :], yT[:, k, tks], wd[:, k, hh * 512:(hh + 1) * 512], k == 0, k == 21, ["yT", "wd"], ["ps%d" % pi])
                    tt("dve", x2t[b2][:, hh * 512:(hh + 1) * 512], PS[pi][:, :], GT(s, 1)[:, hh * 512:(hh + 1) * 512], ALU.mult,
                       ["ps%d" % pi, "GT"], ["x2t%d" % b2])
                tt("pool", x2t[b2], x2t[b2], xq[b2], ALU.add, ["x2t%d" % b2, "xq%d" % b2], ["x2t%d" % b2])
                rstd = rms_tile(x2t[b2], "x2t%d" % b2, None, None)
                stt(ot[b2], x2t[b2], rstd, gfin, ALU.mult, ALU.mult, ["x2t%d" % b2, "rstd", "gfin"], ["ot%d" % b2])
                dma("sp", out[s, tks, :], ot[b2], reads=["ot%d" % b2], writes=["out"])
            S.barrier()

        S.emit()
    return nc


_NC_CACHE = {}


def kernel(x, c, w_ada, b_ada, g_mix, w_in, w_br_moba, w_br_sb, w_out, rel_bias,
           g_ffn, w_up, w_conv, b_conv, w_down, g_final):
    f = lambda a: np.ascontiguousarray(np.asarray(a, dtype=np.float32))
    x = f(x); c = f(c)
    w_ada = f(w_ada)[0]; b_ada = f(b_ada)[0]; g_mix = f(g_mix)[0]; w_in = f(w_in)[0]
    w_bra = f(w_br_moba)[0]; w_brb = f(w_br_sb)[0]; w_out = f(w_out)[0]; rel_bias = f(rel_bias)
    g_ffn = f(g_ffn)[0]; w_up = f(w_up)[0]; w_conv = f(w_conv)[0]; b_conv = f(b_conv)[0]
    w_down = f(w_down)[0]; g_final = f(g_final)
    k = _consts()
    shared = {
        "w_ada": w_ada,
        "b_adaT": f(b_ada.reshape(48, 128).T),
        "b_ada_row": f(np.concatenate([b_ada[2048:3072], b_ada[5120:6144]])[None, :]),
        "g_mixT": f(g_mix.reshape(8, 128).T),
        "g_ffnT": f(g_ffn.reshape(8, 128).T),
        "g_fin": f(g_final[None, :]),
        "w_in": w_in, "w_bra": w_bra, "w_brb": w_brb, "w_out": w_out, "w_up": w_up,
        "w_convT": f(w_conv.T.reshape(44, 128, 3).transpose(1, 0, 2)),
        "b_convT": f(b_conv.reshape(44, 128).T),
        "w_down": w_down,
        "relG": f(rel_bias[:, k["bidx"]]),
        "b31": f(np.broadcast_to(rel_bias[:, 31][None, :], (128, 8))),
        "k_mmask": k["mmask"], "k_mpast": k["mpast"], "k_negm": k["negm"], "k_ident": k["ident"],
        "k_ustrict": k["ustrict"], "k_sel": k["sel"], "k_inv": k["inv"], "k_selrow": k["selrow"],
    }
    in_maps = []
    for i in range(NCORES):
        m = dict(shared)
        m["x"] = f(x[NSEQ * i:NSEQ * (i + 1)])
        m["cT"] = f(c[NSEQ * i:NSEQ * (i + 1)].reshape(NSEQ, 8, 128).transpose(2, 1, 0))
        in_maps.append(m)
    if "nc" not in _NC_CACHE:
        _NC_CACHE["nc"] = build()
    nc = _NC_CACHE["nc"]
    res = run_bass_kernel_spmd(nc, in_maps, core_ids=list(range(NCORES)))
    kernel.last_results = res
    return np.concatenate([np.asarray(r["out"]) for r in res.results], axis=0).astype(np.float32)
```

```python
import contextlib
import numpy as np
import concourse.bass as bass
import concourse.mybir as mybir
from concourse.bass_utils import run_bass_kernel_spmd

F32 = mybir.dt.float32
BF16 = mybir.dt.bfloat16
AF = mybir.ActivationFunctionType
ALU = mybir.AluOpType
AX = mybir.AxisListType

NCORES = 8
SEQ = 2048
D = 1024
NSEQ = 2
DFF = 2816
NEG = -30000.0

DEBUG = False

NDMASEM = 20


class Buf:
    __slots__ = ("name", "w", "r", "rd")

    def __init__(self, name):
        self.name = name
        self.w = None
        self.r = {}
        self.rd = []


class Op:
    __slots__ = ("eng", "fn", "deps", "signal", "semval", "dma", "dsem", "prewait")

    def __init__(self, eng, fn, dma):
        self.eng = eng
        self.fn = fn
        self.deps = []
        self.signal = False
        self.semval = 0
        self.dma = dma
        self.dsem = None
        self.prewait = None


class Sched:
    ENGS = ("pe", "act", "dve", "pool", "sp")

    def __init__(self, nc):
        self.nc = nc
        self.ops = {e: [] for e in self.ENGS}
        self.ndma = {e: 0 for e in self.ENGS}
        self.bufs = {}

    def _B(self, x):
        b = self.bufs.get(x)
        if b is None:
            b = Buf(x)
            self.bufs[x] = b
        return b

    def op(self, eng, fn, reads=(), writes=(), dma=False):
        o = Op(eng, fn, dma)
        deps = []
        for b in reads:
            b = self._B(b)
            if b.w is not None:
                deps.append(b.w)
            if b.name.startswith("ps"):
                for e2, r2 in b.r.items():
                    if e2 != eng:
                        deps.append(r2)
        for b in writes:
            b = self._B(b)
            if b.w is not None:
                deps.append(b.w)
            deps.extend(b.r.values())
            deps.extend(b.rd)
        for b in writes:
            b = self._B(b)
            b.w = o
            b.r = {}
            b.rd = []
        for b in reads:
            b = self._B(b)
            if b.w is not o:
                if dma:
                    b.rd.append(o)
                else:
                    b.r[eng] = o
        seen = set()
        for d in deps:
            if d is o or id(d) in seen:
                continue
            if d.eng == "pe" and eng == "pe" and not d.dma and not dma:
                continue
            seen.add(id(d))
            o.deps.append(d)
            d.signal = True
        if dma:
            k = self.ndma[eng]
            self.ndma[eng] = k + 1
            o.dsem = (eng, k % NDMASEM)
            o.semval = 16 * (k // NDMASEM + 1)
            if k >= NDMASEM:
                o.prewait = (o.dsem, 16 * (k // NDMASEM))
        self.ops[eng].append(o)
        return o

    def barrier(self):
        lasts = []
        for e in self.ENGS:
            for o in reversed(self.ops[e]):
                if not o.dma and o.fn is not None:
                    lasts.append(o)
                    break
            seen = set()
            for o in reversed(self.ops[e]):
                if o.dma and o.dsem not in seen:
                    seen.add(o.dsem)
                    lasts.append(o)
                    if len(seen) >= NDMASEM:
                        break
        for e in self.ENGS:
            o = Op(e, None, False)
            for d in lasts:
                if d.eng == e and not d.dma:
                    continue
                o.deps.append(d)
                d.signal = True
            self.ops[e].append(o)
        for b in self.bufs.values():
            b.w = None
            b.r = {}
            b.rd = []

    def emit(self):
        nc = self.nc
        for e in self.ENGS:
            c = 0
            for o in self.ops[e]:
                if o.dma:
                    continue
                if o.signal:
                    c += 1
                    o.semval = c
        with contextlib.ExitStack() as st:
            esem = {e: st.enter_context(nc.semaphore("s_" + e)) for e in self.ENGS}
            dsem = {}
            for e in self.ENGS:
                for i in range(min(NDMASEM, self.ndma[e])):
                    dsem[(e, i)] = st.enter_context(nc.semaphore("d_%s_%d" % (e, i)))
            block = st.enter_context(nc.Block())

            def run(ename, eng):
                waited = {}

                def wait(key, sem, val):
                    if waited.get(key, 0) >= val:
                        return
                    waited[key] = val
                    eng.wait_ge(sem, val)

                for o in self.ops[ename]:
                    if o.prewait is not None:
                        ds, v = o.prewait
                        wait(ds, dsem[ds], v)
                    for d in o.deps:
                        if d.dma:
                            wait(d.dsem, dsem[d.dsem], d.semval)
                        else:
                            wait(d.eng, esem[d.eng], d.semval)
                    if o.fn is None:
                        continue
                    ins = o.fn(eng)
                    if o.dma:
                        ins.then_inc(dsem[o.dsem], 16)
                    elif o.signal:
                        ins.then_inc(esem[ename], 1)

            @block.tensor
            def _(pe):
                run("pe", pe)

            @block.scalar
            def _(act):
                run("act", act)

            @block.vector
            def _(dve):
                run("dve", dve)

            @block.gpsimd
            def _(pool):
                run("pool", pool)

            @block.sync
            def _(sp):
                run("sp", sp)


def _bucket_table(nmax):
    n = np.arange(nmax, dtype=np.float64)
    nf = np.maximum(n, 16.0)
    large = 16 + np.floor(np.log(nf / 16.0) / np.log(8.0) * 16.0 + 1e-9).astype(np.int64)
    large = np.minimum(large, 31)
    return np.where(n < 16, n.astype(np.int64), large)


def _consts():
    i = np.arange(128)[:, None]
    c = np.arange(1024)[None, :]
    rel = c - 384 - i
    bt = _bucket_table(1024)
    bidx = bt[np.maximum(rel, 0)]
    mmask = np.where(rel < 0, 8.0 * NEG, 0.0).astype(np.float32)
    mpast = (rel > 0).astype(np.float32)
    negm = np.where(rel > 0, 0.0, NEG).astype(np.float32)
    ident = np.eye(128, dtype=np.float32)
    ustrict = (np.arange(128)[:, None] > np.arange(128)[None, :]).astype(np.float32)
    sel = np.zeros((16, 16, 128), np.float32)
    for w in range(16):
        sel[w, w, :] = 1.0
    sel = sel.reshape(16, 16 * 128)
    inv = np.zeros((16, 2, 8), np.float32)
    for t in range(16):
        inv[t, :, (t // 2):] = 1.0
    inv = np.broadcast_to(inv.reshape(1, 256), (128, 256)).copy()
    selrow = np.zeros((2, 2, 128), np.float32)
    selrow[0, 0, :] = 1.0
    selrow[1, 1, :] = 1.0
    selrow = selrow.reshape(2, 256)
    return dict(bidx=bidx, mmask=mmask, mpast=mpast, negm=negm, ident=ident, ustrict=ustrict,
                sel=sel, inv=inv, selrow=selrow)


def build(stop_after=None):
    nc = bass.Bass("TRN2", target_bir_lowering=False)

    def din(name, shape, dt=F32):
        return nc.dram_tensor(name, list(shape), dt, kind="ExternalInput").ap()

    x = din("x", [NSEQ, SEQ, D])
    cT = din("cT", [128, 8, 2])
    w_ada = din("w_ada", [D, 6 * D])
    b_adaT = din("b_adaT", [128, 48])
    b_ada_row = din("b_ada_row", [1, 2048])
    g_mixT = din("g_mixT", [128, 8])
    g_ffnT = din("g_ffnT", [128, 8])
    g_fin = din("g_fin", [1, D])
    w_in = din("w_in", [D, 5120])
    w_bra = din("w_bra", [512, D])
    w_brb = din("w_brb", [512, D])
    w_out = din("w_out", [D, D])
    w_up = din("w_up", [D, 2 * DFF])
    w_convT = din("w_convT", [128, 44, 3])
    b_convT = din("b_convT", [128, 44])
    w_down = din("w_down", [DFF, D])
    relG = din("relG", [8, 128, 1024])
    b31 = din("b31", [128, 8])
    k_mmask = din("k_mmask", [128, 1024])
    k_mpast = din("k_mpast", [128, 1024])
    k_negm = din("k_negm", [128, 1024])
    k_ident = din("k_ident", [128, 128])
    k_ustrict = din("k_ustrict", [128, 128])
    k_sel = din("k_sel", [16, 2048])
    k_inv = din("k_inv", [128, 256])
    k_selrow = din("k_selrow", [2, 256])
    out = nc.dram_tensor("out", [NSEQ, SEQ, D], F32, kind="ExternalOutput").ap()
    x1s = nc.dram_tensor("x1s", [NSEQ, SEQ, D], F32, kind="Internal").ap()
    dbg = {}
    if DEBUG:
        dbg["hT"] = nc.dram_tensor("dbg_hT", [128, 8 * SEQ], BF16, kind="ExternalOutput").ap()
        dbg["oT"] = nc.dram_tensor("dbg_oT", [128, 8 * SEQ], BF16, kind="ExternalOutput").ap()
        dbg["combT"] = nc.dram_tensor("dbg_combT", [128, 8 * SEQ], BF16, kind="ExternalOutput").ap()
        dbg["h2T"] = nc.dram_tensor("dbg_h2T", [128, 8 * SEQ], BF16, kind="ExternalOutput").ap()
        dbg["yT"] = nc.dram_tensor("dbg_yT", [128, 22 * SEQ], BF16, kind="ExternalOutput").ap()
        dbg["misc"] = nc.dram_tensor("dbg_misc", [128, 1024], F32, kind="ExternalOutput").ap()

    K = 1024
    TOTAL = 207 * K
    with contextlib.ExitStack() as st:
        big = st.enter_context(nc.sbuf_tensor("big", [128, TOTAL // 4], F32))
        PS = [st.enter_context(nc.psum_tensor("ps%d" % i, [128, 512], F32)) for i in range(8)]
        S = Sched(nc)

        def VW(off, nbytes, dt=F32, parts=128):
            assert off % 4 == 0 and nbytes % 4 == 0 and off + nbytes <= TOTAL, (off, nbytes)
            v = big[0:parts, off // 4:(off + nbytes) // 4]
            if dt == BF16:
                v = v.bitcast(BF16)
            return v

        def psb(i):
            return PS[i][:, :].bitcast(BF16)

        A_OFF = 0
        B_OFF = 32 * K
        C_OFF = 64 * K
        Y_OFF = 32 * K
        D_OFF = 120 * K
        D_SZ = 71 * K
        CO = 191 * K

        hT = VW(A_OFF, 32 * K, BF16).rearrange("p (k n) -> p k n", k=8)
        oT = VW(B_OFF, 32 * K, BF16).rearrange("p (k n) -> p k n", k=8)
        combT = VW(C_OFF, 32 * K, BF16).rearrange("p (k n) -> p k n", k=8)
        Bh = VW(C_OFF, 16 * K, BF16).rearrange("p (h n) -> p h n", h=8)
        yT = VW(Y_OFF, 88 * K, BF16).rearrange("p (k n) -> p k n", k=22)

        co = [CO]

        def calloc(nbytes, dt=F32, parts=128):
            v = VW(co[0], nbytes, dt, parts)
            co[0] += (nbytes + 31) // 32 * 32
            assert co[0] <= TOTAL
            return v

        identb = calloc(256, BF16)
        ustrict = calloc(256, BF16)
        onesb = calloc(256, BF16)
        onesf = calloc(512, F32)
        selb = calloc(4096, BF16)
        selrow = calloc(1024, F32)
        mpast = calloc(2048, BF16)
        negm = calloc(2048, BF16)
        inv = calloc(1024, F32)
        neginv = calloc(1024, F32)
        b31s = calloc(32, F32)
        modT = calloc(384, F32).rearrange("p (c b) -> p c b", b=2)
        A1 = calloc(64, F32).rearrange("p (c b) -> p c b", b=2)
        A2 = calloc(64, F32).rearrange("p (c b) -> p c b", b=2)
        gmixT = calloc(32, F32)
        gffnT = calloc(32, F32)
        wconv = calloc(44 * 3 * 4, F32).rearrange("p (c i) -> p c i", i=3)
        bconv = calloc(44 * 4, F32)
        badaT = calloc(48 * 4, F32)
        small = calloc(256, F32)
        CONST_END = co[0]

        def dma(eng, out_, in_, reads=(), writes=()):
            return S.op(eng, lambda e: e.dma_start(out=out_, in_=in_), reads=reads, writes=writes, dma=True)

        def mm(o, l, r, start, stop, reads, writes):
            return S.op("pe", lambda e: e.matmul(o, lhsT=l, rhs=r, start=start, stop=stop), reads=reads, writes=writes)

        def act(o, i, func, reads, writes, bias=None, scale=None, accum=None):
            kw = {}
            if bias is not None:
                kw["bias"] = bias
            if scale is not None:
                kw["scale"] = scale
            if accum is not None:
                kw["accum_out"] = accum
            return S.op("act", lambda e: e.activation(out=o, in_=i, func=func, **kw), reads=reads, writes=writes)

        def tsc(eng, o, i, s1, s2, op0, op1, reads, writes):
            if s2 is None:
                return S.op(eng, lambda e: e.tensor_scalar(out=o, in0=i, scalar1=s1, scalar2=None, op0=op0), reads=reads, writes=writes)
            return S.op(eng, lambda e: e.tensor_scalar(out=o, in0=i, scalar1=s1, scalar2=s2, op0=op0, op1=op1), reads=reads, writes=writes)

        def tt(eng, o, a, b, op, reads, writes):
            return S.op(eng, lambda e: e.tensor_tensor(out=o, in0=a, in1=b, op=op), reads=reads, writes=writes)

        def stt(o, a, s, b, op0, op1, reads, writes, accum=None):
            if accum is None:
                return S.op("dve", lambda e: e.scalar_tensor_tensor(out=o, in0=a, scalar=s, in1=b, op0=op0, op1=op1), reads=reads, writes=writes)
            return S.op("dve", lambda e: e.scalar_tensor_tensor(out=o, in0=a, scalar=s, in1=b, op0=op0, op1=op1, accum_out=accum), reads=reads, writes=writes)

        def cp(eng, o, i, reads, writes):
            if eng == "act":
                return S.op("act", lambda e: e.activation(out=o, in_=i, func=AF.Identity), reads=reads, writes=writes)
            return S.op(eng, lambda e: e.tensor_copy(out=o, in_=i), reads=reads, writes=writes)

        stg = VW(D_OFF, 4096, F32)
        stg2 = VW(D_OFF + 4096, 4096, F32)
        dma("sp", stg[:, 0:128], k_ident, writes=["stg"])
        cp("dve", identb, stg[:, 0:128], ["stg"], ["identb"])
        dma("sp", stg[:, 128:256], k_ustrict, writes=["stgb"])
        cp("dve", ustrict, stg[:, 128:256], ["stgb"], ["ustrict"])
        S.op("pool", lambda e: e.memset(onesb, 1.0), writes=["onesb"])
        S.op("pool", lambda e: e.memset(onesf, 1.0), writes=["onesf"])
        dma("pool", selb[0:16, :], k_sel, writes=["selb"])
        dma("sp", selrow[0:2, :], k_selrow, writes=["selrow"])
        dma("pool", mpast, k_mpast, writes=["mpast"])
        dma("pool", negm, k_negm, writes=["negm"])
        dma("sp", inv, k_inv, writes=["inv"])
        tsc("dve", neginv, inv, -1e30, None, ALU.mult, None, ["inv"], ["neginv"])
        dma("sp", b31s, b31, writes=["b31s"])
        dma("sp", gmixT, g_mixT, writes=["gmixT"])
        dma("sp", gffnT, g_ffnT, writes=["gffnT"])
        dma("sp", wconv, w_convT, writes=["wconv"])
        dma("sp", bconv, b_convT, writes=["bconv"])
        dma("sp", badaT, b_adaT, writes=["badaT"])

        cTt = VW(D_OFF + 8192, 64, F32).rearrange("p (k b) -> p k b", b=2)
        cactb = VW(D_OFF + 8192 + 64, 32, BF16).rearrange("p (k b) -> p k b", b=2)
        cactf = VW(D_OFF + 8192 + 128, 64, F32).rearrange("p (k b) -> p k b", b=2)
        dma("sp", cTt, cT, writes=["cTt"])
        act(cactb, cTt, AF.Silu, ["cTt"], ["cactb"])
        act(cactf, cTt, AF.Silu, ["cTt"], ["cactf"])
        gtrow = VW(D_OFF + 8192 + 256, 8192, F32, parts=2)
        badar = VW(D_OFF + 8192 + 256 + 8192, 8192, F32, parts=2)
        dma("sp", badar[0:1, :], b_ada_row, writes=["badar0"])
        dma("sp", badar[1:2, :], b_ada_row, writes=["badar1"])
        WB_OFF = D_OFF + 32 * K
        wada_v = w_ada.rearrange("(k p) n -> p k n", p=128)
        for blk in range(8):
            wb = VW(WB_OFF + (blk % 2) * 12 * K, 12 * K, BF16).rearrange("p (k n) -> p k n", k=8)
            wn = "wada%d" % (blk % 2)
            dma("pool", wb, wada_v[:, :, blk * 768:(blk + 1) * 768], writes=[wn])
            for j in range(6):
                cidx = blk * 6 + j
                for k in range(8):
                    mm(PS[0][:, 2 * cidx:2 * cidx + 2], wb[:, k, j * 128:(j + 1) * 128], cactb[:, k, :], k == 0, k == 7,
                       [wn, "cactb"], ["ps0"])
        tt("dve", modT, PS[0][:, 0:96].rearrange("p (c b) -> p c b", b=2),
           badaT.rearrange("p (c o) -> p c o", o=1).to_broadcast([128, 48, 2]), ALU.add, ["ps0", "badaT"], ["modT"])
        tsc("dve", A1, modT[:, 8:16, :], 1.0, None, ALU.add, None, ["modT"], ["A1"])
        tt("dve", A1, A1, gmixT.rearrange("p (c o) -> p c o", o=1).to_broadcast([128, 8, 2]), ALU.mult, ["A1", "gmixT"], ["A1"])
        tsc("dve", A2, modT[:, 32:40, :], 1.0, None, ALU.add, None, ["modT"], ["A2"])
        tt("dve", A2, A2, gffnT.rearrange("p (c o) -> p c o", o=1).to_broadcast([128, 8, 2]), ALU.mult, ["A2", "gffnT"], ["A2"])
        GT_OFF = D_OFF + 32 * K + 24 * K
        S.barrier()
        wf = VW(D_OFF + 26 * K, 32 * K, F32).rearrange("p (k n) -> p k n", k=8)
        for g, c0 in enumerate((2048, 5120)):
            dma("sp", wf, wada_v[:, :, c0:c0 + 1024], writes=["wf"])
            for hh in range(2):
                for k in range(8):
                    mm(PS[1][0:2, :], cactf[:, k, :], wf[:, k, hh * 512:(hh + 1) * 512], k == 0, k == 7, ["wf", "cactf"], ["ps1"])
                tt("dve", gtrow[0:2, g * 1024 + hh * 512:g * 1024 + (hh + 1) * 512], PS[1][0:2, :],
                   badar[0:2, g * 1024 + hh * 512:g * 1024 + (hh + 1) * 512], ALU.add, ["ps1", "badar0", "badar1"], ["gtrow"])
        GTB = D_OFF + D_SZ - 16 * K

        def GT(s, g):
            return VW(GTB + (s * 2 + g) * 4096, 4096, F32)

        for s in range(NSEQ):
            for g in range(2):
                for hh in range(2):
                    mm(PS[2][:, :], selrow[0:2, s * 128:(s + 1) * 128], gtrow[0:2, g * 1024 + hh * 512:g * 1024 + (hh + 1) * 512],
                       True, True, ["selrow", "gtrow"], ["ps2"])
                    cp("act", GT(s, g)[:, hh * 512:(hh + 1) * 512], PS[2][:, :], ["ps2"], ["GT"])
        S.barrier()
        STOP = [False]

        def stop(tag):
            if stop_after == tag:
                STOP[0] = True
            return STOP[0]
        DW = GTB - D_OFF

        def rms_tile(xt_ap, xtn, ssn, tag):
            junk = VW(D_OFF + 0, 4096, F32)
            ss = small[:, 0:1]
            lnv = small[:, 1:2]
            rstd = small[:, 2:3]
            stt(junk, xt_ap, 1.0, xt_ap, ALU.mult, ALU.mult, [xtn], ["junk", "ss"], accum=ss)
            act(lnv, ss, AF.Ln, ["ss"], ["lnv"], bias=1e-6, scale=1.0 / 1024.0)
            act(rstd, lnv, AF.Exp, ["lnv"], ["rstd"], scale=-0.5)
            return rstd

        def norm_to_T(xt_ap, xtn, dstT, Amod, Bmod_c0, s, t, xnb, xnbn, psi):
            rstd = rms_tile(xt_ap, xtn, None, None)
            tsc("dve", xnb, xt_ap, rstd, None, ALU.mult, None, [xtn, "rstd"], [xnbn])
            pn = "ps%d" % psi
            pv = psb(psi)
            for c in range(8):
                S.op("pe", lambda e, c=c: e.transpose(out=pv[:, c * 128:(c + 1) * 128], in_=xnb[:, c * 128:(c + 1) * 128], identity=identb),
                     reads=[xnbn, "identb"], writes=[pn])
            for c in range(8):
                o = dstT[:, c, t * 128:(t + 1) * 128]
                i = pv[:, c * 128:(c + 1) * 128]
                if t % 2 == 0:
                    act(o, i, AF.Identity, [pn, "A1", "A2", "modT"], ["dstT"], bias=modT[:, Bmod_c0 + c, s:s + 1], scale=Amod[:, c, s:s + 1])
                else:
                    tsc("dve", o, i, Amod[:, c, s:s + 1], modT[:, Bmod_c0 + c, s:s + 1], ALU.mult, ALU.add, [pn, "A1", "A2", "modT"], ["dstT"])

        w_in_v = w_in.rearrange("(k p) n -> p k n", p=128)

        for s in range(NSEQ):
            if stop("setup"):
                break
            for t in range(16):
                xt = VW(D_OFF + 4096 + (t % 2) * 4096, 4096, F32)
                xtn = "xt%d" % (t % 2)
                dma("sp", xt, x[s, t * 128:(t + 1) * 128, :], writes=[xtn])
                xnb = VW(D_OFF + 12288 + (t % 2) * 2048, 2048, BF16)
                norm_to_T(xt, xtn, hT, A1, 0, s, t, xnb, "xnb%d" % (t % 2), 6 + (t % 2))
            S.barrier()
            if DEBUG and s == 0:
                dma("sp", dbg["hT"], VW(A_OFF, 32 * K, BF16), reads=["dstT"])
                S.barrier()

            if stop("p1"):
                break
            mm_f = VW(D_OFF, 4096, F32)
            dma("sp", mm_f, k_mmask, writes=["mm_f"])
            for h in range(8):
                gtile = VW(D_OFF + 4096 + (h % 2) * 4096, 4096, F32)
                gn = "gt%d" % (h % 2)
                dma("sp", gtile, relG[h], writes=[gn])
                tsc("dve", gtile, gtile, b31s[:, h:h + 1], 8.0, ALU.subtract, ALU.mult, [gn, "b31s"], [gn])
                tt("dve", Bh[:, h, :], gtile, mm_f, ALU.add, [gn, "mm_f"], ["Bh"])
            S.barrier()
            o_ = [D_OFF]

            def dalloc(nbytes, dt=F32, parts=128):
                v = VW(o_[0], nbytes, dt, parts)
                o_[0] += (nbytes + 31) // 32 * 32
                assert o_[0] <= GTB, o_[0] - GTB
                return v

            wq = [dalloc(2048, BF16).rearrange("p (k n) -> p k n", k=8) for _ in range(1)]
            wk = [dalloc(2048, BF16).rearrange("p (k n) -> p k n", k=8) for _ in range(1)]
            wv = [dalloc(2048, BF16).rearrange("p (k n) -> p k n", k=8) for _ in range(1)]
            qT = dalloc(4096, BF16)
            kT = dalloc(4096, BF16)
            Vaug = dalloc(8192, BF16).rearrange("p (t n) -> p t n", t=16)
            negT = dalloc(4096, BF16)
            gm = dalloc(1024, F32)
            top8 = dalloc(1024, F32)
            mge = dalloc(1024, F32)
            negq = dalloc(512, BF16)
            kms = dalloc(32, F32)
            kmh = dalloc(16, BF16)
            kmr = dalloc(32, F32)
            kml = dalloc(16, BF16)
            PT = [dalloc(1024, BF16) for _ in range(3)]
            e_t = [dalloc(2048, F32) for _ in range(2)]
            sp_t = [dalloc(2048, F32) for _ in range(2)]
            lkm = [dalloc(1024, BF16) for _ in range(2)]
            ssum = [dalloc(1024, BF16) for _ in range(2)]
            t_t = [dalloc(2048, F32) for _ in range(2)]
            a_t = [dalloc(1024, BF16) for _ in range(2)]
            rr = e_t[0]
            lnr = e_t[1]
            bcs = sp_t[0]

            S.op("pool", lambda e: e.memset(Vaug, 0.0), writes=["Vaug"])
            S.op("pool", lambda e: e.memset(Vaug[:, :, 64:65], 1.0), reads=[], writes=["Vaug"])
            S.op("pool", lambda e: e.memset(Vaug[:, :, 128:129], 1.0), reads=[], writes=["Vaug"])

            zc = [0]

            def zbank():
                i = zc[0] % 3
                zc[0] += 1
                return i

            def project_pair(kind, p):
                base = 0 if kind == 0 else 1536
                pp = 0
                dma("pool", wq[pp], w_in_v[:, :, base + 128 * p:base + 128 * p + 128], writes=["wq%d" % pp])
                dma("pool", wk[pp], w_in_v[:, :, base + 512 + 128 * p:base + 512 + 128 * p + 128], writes=["wk%d" % pp])
                dma("pool", wv[pp], w_in_v[:, :, base + 1024 + 128 * p:base + 1024 + 128 * p + 128], writes=["wv%d" % pp])
                for (wt, wn, dst, dn) in ((wq[pp], "wq%d" % pp, qT, "qT"), (wk[pp], "wk%d" % pp, kT, "kT")):
                    for tq in range(4):
                        zi = zbank()
                        for k in range(8):
                            mm(PS[zi][:, :], wt[:, k, :], hT[:, k, tq * 512:(tq + 1) * 512], k == 0, k == 7, [wn, "hT"], ["ps%d" % zi])
                        cp("act" if tq % 2 == 0 else "dve", dst[:, tq * 512:(tq + 1) * 512], PS[zi][:, :], ["ps%d" % zi], [dn])
                for g in range(4):
                    zi = zbank()
                    for tl in range(4):
                        t = g * 4 + tl
                        for k in range(8):
                            mm(PS[zi][:, tl * 128:(tl + 1) * 128], hT[:, k, t * 128:(t + 1) * 128], wv[pp][:, k, :], k == 0, k == 7,
                               ["wv%d" % pp, "hT"], ["ps%d" % zi])
                    pv3 = PS[zi][:, :].rearrange("p (t n) -> p t n", t=4)
                    cp("act", Vaug[:, g * 4:(g + 1) * 4, 0:64], pv3[:, :, 0:64], ["ps%d" % zi], ["Vaug"])
                    cp("dve", Vaug[:, g * 4:(g + 1) * 4, 192:256], pv3[:, :, 64:128], ["ps%d" % zi], ["Vaug"])

            def moba_gate():
                tensor_reduce = lambda e: e.tensor_reduce(out=kms, in_=kT.rearrange("p (n j) -> p n j", j=256), axis=AX.X, op=ALU.add)
                S.op("dve", tensor_reduce, reads=["kT"], writes=["kms"])
                tsc("dve", kms, kms, 1.0 / 256.0, None, ALU.mult, None, ["kms"], ["kms"])
                cp("dve", kmh, kms, ["kms"], ["kmh"])
                tt("dve", kmr, kms, kmh, ALU.subtract, ["kms", "kmh"], ["kmr"])
                cp("dve", kml, kmr, ["kmr"], ["kml"])
                gm4 = gm.rearrange("p (t h n) -> p t h n", t=16, h=2)
                ni4 = neginv.rearrange("p (t h n) -> p t h n", t=16, h=2)
                for hd in range(2):
                    hs = slice(64 * hd, 64 * hd + 64)
                    gb = 3 + hd
                    gv = PS[gb][:, 0:128].rearrange("p (t n) -> p t n", t=16)
                    for t in range(16):
                        mm(gv[:, t, :], qT[hs, t * 128:(t + 1) * 128], kmh[hs, :], True, False, ["qT", "kmh"], ["ps%d" % gb])
                        mm(gv[:, t, :], qT[hs, t * 128:(t + 1) * 128], kml[hs, :], False, True, ["qT", "kml"], ["ps%d" % gb])
                    tt("dve", gm4[:, :, hd, :], gv, ni4[:, :, hd, :], ALU.add, ["ps%d" % gb, "neginv"], ["gm"])
                gm3 = gm.rearrange("p (g n) -> p g n", n=8)
                t83 = top8.rearrange("p (g n) -> p g n", n=8)
                for g in range(32):
                    S.op("dve", lambda e, g=g: e.max(out=t83[:, g, :], in_=gm3[:, g, :]), reads=["gm"], writes=["top8"])
                tt("dve", mge.rearrange("p (g n) -> p g n", n=8), gm3, t83[:, :, 2:3].to_broadcast([128, 32, 8]), ALU.is_ge,
                   ["gm", "top8"], ["mge"])
                tt("dve", mge, mge, inv, ALU.max, ["mge", "inv"], ["mge"])
                tsc("dve", negq, mge, -8.0 * NEG, 8.0 * NEG, ALU.mult, ALU.add, ["mge"], ["negq"])
                for half in range(2):
                    pv = psb(4)
                    for tl in range(8):
                        t = half * 8 + tl
                        S.op("pe", lambda e, t=t, tl=tl, pv=pv: e.transpose(out=pv[0:16, tl * 128:(tl + 1) * 128], in_=negq[:, t * 16:(t + 1) * 16], identity=identb),
                             reads=["negq", "identb"], writes=["ps4"])
                    cp("dve", negT[0:16, half * 1024:(half + 1) * 1024], pv[0:16, :], ["ps4"], ["negT"])

            acc_c = [0]

            def moba_head(p, hd):
                head = 2 * p + hd
                hs = slice(64 * hd, 64 * hd + 64)
                pending = [None]
                for qt in range(4):
                    ai = 5 + (acc_c[0] % 2)
                    acc_c[0] += 1
                    an = "ps%d" % ai
                    nch = 4 * qt + 4
                    qs = slice(qt * 512, (qt + 1) * 512)

                    def scores(kc):
                        zi = zbank()
                        zn = "ps%d" % zi
                        n = kc // 2
                        d0 = 512 * qt - 128 * kc
                        use_sel = n <= 2 * qt
                        use_toe = d0 < 240
                        mm(PS[zi][:, :], kT[hs, kc * 128:(kc + 1) * 128], qT[hs, qs], True, not (use_sel or use_toe), ["kT", "qT"], [zn])
                        if use_sel:
                            w = hd * 8 + n
                            mm(PS[zi][:, :], selb[0:16, w * 128:(w + 1) * 128], negT[0:16, qs], False, not use_toe, ["selb", "negT"], [zn])
                        if use_toe:
                            c0 = d0 + 384
                            mm(PS[zi][:, :], identb, Bh[:, head, c0:c0 + 512], False, True, ["identb", "Bh"], [zn])
                        pi = kc % 3
                        act(PT[pi], PS[zi][:, :], AF.Exp, [zn, "b31s"], ["PT%d" % pi], bias=b31s[:, head:head + 1], scale=0.125)

                    def pv(kc):
                        pi = kc % 3
                        if hd == 0:
                            mm(PS[ai][0:65, :], Vaug[:, kc, 0:65], PT[pi], kc == 0, kc == nch - 1, ["Vaug", "PT%d" % pi], [an])
                        else:
                            mm(PS[ai][:, :], Vaug[:, kc, 128:256], PT[pi], kc == 0, kc == nch - 1, ["Vaug", "PT%d" % pi], [an])

                    def finalize(ai=ai, an=an, qs=qs):
                        srow = 64 if hd == 0 else 0
                        act(lnr[0:1, 0:512], PS[ai][srow:srow + 1, :], AF.Ln, [an], ["e1"])
                        act(rr[0:1, 0:512], lnr[0:1, 0:512], AF.Exp, ["e1"], ["e0"], scale=-1.0)
                        mm(PS[7][:, :], onesf[0:1, :], rr[0:1, 0:512], True, True, ["onesf", "e0"], ["ps7"])
                        cp("act", bcs[hs, 0:512], PS[7][hs, :], ["ps7"], ["sp0"])
                        tt("dve", oT[hs, p, qs], PS[ai][hs, :], bcs[hs, 0:512], ALU.mult, [an, "sp0"], ["oT"])

                    for kc in range(nch + 1):
                        if kc < nch:
                            scores(kc)
                        if kc >= 1:
                            pv(kc - 1)
                    finalize()

            c2 = [C_OFF + 16 * K]

            def c2alloc(nbytes, dt=F32):
                v = VW(c2[0], nbytes, dt)
                c2[0] += nbytes
                assert c2[0] <= C_OFF + 32 * K
                return v

            SBS = [dict(e=e_t, sp=sp_t, lkm=lkm, ssum=ssum, t=t_t, a=a_t, sfx="", L=3, acc=5),
                   dict(e=[c2alloc(2048) for _ in range(2)], sp=[c2alloc(2048) for _ in range(2)],
                        lkm=[c2alloc(1024, BF16) for _ in range(2)], ssum=[c2alloc(1024, BF16) for _ in range(2)],
                        t=[c2alloc(2048) for _ in range(2)], a=[dalloc(1024, BF16) for _ in range(2)], sfx="b", L=4, acc=6)]

            accrot = [0]

            def sb_chunk(p, hd, qt, idx, ai):
                B = SBS[hd]
                sx = B["sfx"]
                hs = slice(64 * hd, 64 * hd + 64)
                li = B["L"]
                ln_ = "ps%d" % li
                an = "ps%d" % ai
                nch = 4 * qt + 4
                qs = slice(qt * 512, (qt + 1) * 512)
                kc = nch - 1 - idx
                b2 = idx % 2
                d0 = 512 * qt - 128 * kc
                diag = kc >= 4 * qt
                c0 = d0 + 384
                en, spn, lkn, tn, an_ = ("e%d%s" % (b2, sx), "sp%d%s" % (b2, sx), "lkm%d%s" % (b2, sx), "t%d%s" % (b2, sx), "a%d%s" % (b2, sx))
                e_b, sp_b, lk_b, t_b, a_b = B["e"][b2], B["sp"][b2], B["lkm"][b2], B["t"][b2], B["a"][b2]
                zi = zbank()
                zn = "ps%d" % zi
                mm(PS[zi][:, :], kT[hs, kc * 128:(kc + 1) * 128], qT[hs, qs], True, True, ["kT", "qT"], [zn])
                act(e_b, PS[zi][:, :], AF.Exp, [zn], [en], scale=-0.125)
                act(sp_b, e_b, AF.Ln, [en], [spn], bias=1.0)
                stt(lk_b, PS[zi][:, :], 0.125, sp_b, ALU.mult, ALU.add, [zn, spn], [lkn])
                if diag:
                    tt("pool", lk_b, lk_b, mpast[:, c0:c0 + 512], ALU.mult, [lkn, "mpast"], [lkn])
                yield
                mm(PS[li][:, :], ustrict, lk_b, True, idx == 0, ["ustrict", lkn], [ln_])
                if idx > 0:
                    mm(PS[li][:, :], onesb, B["ssum"][(idx - 1) % 2], False, True, ["onesb", "ssum%d%s" % ((idx - 1) % 2, sx)], [ln_])
                if kc > 0:
                    if idx == 0:
                        cp("pool", B["ssum"][0], lk_b, [lkn], ["ssum0%s" % sx])
                    else:
                        tt("pool", B["ssum"][idx % 2], B["ssum"][(idx - 1) % 2], lk_b, ALU.add,
                           ["ssum%d%s" % ((idx - 1) % 2, sx), lkn], ["ssum%d%s" % (idx % 2, sx)])
                stt(t_b, PS[li][:, :], -1.0, sp_b, ALU.mult, ALU.subtract, [ln_, spn], [tn])
                if diag:
                    tt("pool", t_b, t_b, negm[:, c0:c0 + 512], ALU.add, [tn, "negm"], [tn])
                yield
                act(a_b, t_b, AF.Exp, [tn], [an_])
                if hd == 0:
                    mm(PS[ai][0:64, :], Vaug[:, kc, 0:64], a_b, idx == 0, kc == 0, ["Vaug", an_], [an])
                else:
                    mm(PS[ai][:, :], Vaug[:, kc, 128:256], a_b, idx == 0, kc == 0, ["Vaug", an_], [an])
                if kc == 0:
                    cp("act", oT[hs, 4 + p, qs], PS[ai][hs, :], [an], ["oT"])

            def sb_stream(p, hd):
                for qt in range(4):
                    ai = 5 + (accrot[0] % 3)
                    accrot[0] += 1
                    for idx in range(4 * qt + 4):
                        yield sb_chunk(p, hd, qt, idx, ai)

            def sb_pair(p):
                streams = [sb_stream(p, 0), sb_stream(p, 1)]
                active = []
                more = True
                while more or active:
                    for g in list(active):
                        try:
                            next(g)
                        except StopIteration:
                            active.remove(g)
                    more = False
                    for st_ in streams:
                        g = next(st_, None)
                        if g is not None:
                            more = True
                            next(g)
                            active.append(g)

            for kind in range(2):
                for p in range(4):
                    project_pair(kind, p)
                    if kind == 0:
                        moba_gate()
                        for hd in range(2):
                            moba_head(p, hd)
                    else:
                        sb_pair(p)
            S.barrier()
            if DEBUG and s == 0:
                dma("sp", dbg["oT"], VW(B_OFF, 32 * K, BF16), reads=["oT"])
                S.barrier()

            if stop("p2"):
                break
            o_[0] = D_OFF
            wbra = dalloc(8192, BF16).rearrange("p (k n) -> p k n", k=4)
            wbrb = dalloc(8192, BF16).rearrange("p (k n) -> p k n", k=4)
            wg = dalloc(16 * K, BF16).rearrange("p (k n) -> p k n", k=8)
            sg = [dalloc(2048, F32) for _ in range(2)]
            t1 = [dalloc(2048, F32) for _ in range(2)]
            t2 = [dalloc(2048, F32) for _ in range(2)]
            dma("pool", wbra, w_bra.rearrange("(k p) n -> p k n", p=128), writes=["wbra"])
            dma("pool", wbrb, w_brb.rearrange("(k p) n -> p k n", p=128), writes=["wbrb"])
            cnt = 0
            for cg in range(2):
                for j in range(4):
                    c = cg * 4 + j
                    dma("pool", wg[:, :, j * 256:j * 256 + 128], w_in_v[:, :, 3072 + 128 * c:3072 + 128 * c + 128], reads=[], writes=["wga%d" % j])
                    dma("pool", wg[:, :, j * 256 + 128:j * 256 + 256], w_in_v[:, :, 4096 + 128 * c:4096 + 128 * c + 128], reads=[], writes=["wgb%d" % j])
                for j in range(4):
                    c = cg * 4 + j
                    for tq in range(4):
                        ts_ = slice(tq * 512, (tq + 1) * 512)
                        b2 = cnt % 2
                        cnt += 1
                        pa, pb, pga, pgb = (0, 1, 2, 3) if b2 == 0 else (4, 5, 6, 7)
                        for k in range(4):
                            mm(PS[pa][:, :], wbra[:, k, c * 128:(c + 1) * 128], oT[:, k, ts_], k == 0, k == 3, ["wbra", "oT"], ["ps%d" % pa])
                        for k in range(4):
                            mm(PS[pb][:, :], wbrb[:, k, c * 128:(c + 1) * 128], oT[:, 4 + k, ts_], k == 0, k == 3, ["wbrb", "oT"], ["ps%d" % pb])
                        for k in range(8):
                            mm(PS[pga][:, :], wg[:, k, j * 256:j * 256 + 128], hT[:, k, ts_], k == 0, k == 7, ["wga%d" % j, "hT"], ["ps%d" % pga])
                        for k in range(8):
                            mm(PS[pgb][:, :], wg[:, k, j * 256 + 128:j * 256 + 256], hT[:, k, ts_], k == 0, k == 7, ["wgb%d" % j, "hT"], ["ps%d" % pgb])
                        act(sg[0], PS[pga][:, :], AF.Sigmoid, ["ps%d" % pga], ["sg0"])
                        act(sg[1], PS[pgb][:, :], AF.Sigmoid, ["ps%d" % pgb], ["sg1"])
                        tt("dve", t1[b2], PS[pa][:, :], sg[0], ALU.mult, ["ps%d" % pa, "sg0"], ["t1%d" % b2])
                        tt("dve", t2[b2], PS[pb][:, :], sg[1], ALU.mult, ["ps%d" % pb, "sg1"], ["t2%d" % b2])
                        tt("pool", combT[:, c, ts_], t1[b2], t2[b2], ALU.add, ["t1%d" % b2, "t2%d" % b2], ["combT"])
            S.barrier()
            if DEBUG and s == 0:
                dma("sp", dbg["combT"], VW(C_OFF, 32 * K, BF16), reads=["combT"])
                S.barrier()

            if stop("p3a"):
                break
            o_[0] = D_OFF + 4096
            wo = dalloc(16 * K, BF16).rearrange("p (k n) -> p k n", k=8)
            xts = [dalloc(4096, F32) for _ in range(2)]
            x1t = [dalloc(4096, F32) for _ in range(2)]
            xnbs = [dalloc(2048, BF16) for _ in range(2)]
            dma("pool", wo, w_out.rearrange("(k p) n -> p k n", p=128), writes=["wo"])
            for t in range(16):
                b2 = t % 2
                tks = slice(t * 128, (t + 1) * 128)
                dma("sp", xts[b2], x[s, tks, :], writes=["xts%d" % b2])
                for hh in range(2):
                    pi = 2 * b2 + hh
                    for k in range(8):
                        mm(PS[pi][:, :], combT[:, k, tks], wo[:, k, hh * 512:(hh + 1) * 512], k == 0, k == 7, ["combT", "wo"], ["ps%d" % pi])
                    tt("dve", x1t[b2][:, hh * 512:(hh + 1) * 512], PS[pi][:, :], GT(s, 0)[:, hh * 512:(hh + 1) * 512], ALU.mult,
                       ["ps%d" % pi, "GT"], ["x1t%d" % b2])
                import os as _os
                if not _os.environ.get("SKIP_ADD"):
                    tt("pool", x1t[b2], x1t[b2], xts[b2], ALU.add, ["x1t%d" % b2, "xts%d" % b2], ["x1t%d" % b2])
                if not _os.environ.get("SKIP_STORE"):
                    dma("sp", x1s[s, tks, :], x1t[b2], reads=["x1t%d" % b2], writes=["x1s"])
                if not _os.environ.get("SKIP_NORM"):
                    norm_to_T(x1t[b2], "x1t%d" % b2, hT, A2, 24, s, t, xnbs[b2], "xnbs%d" % b2, 6 + b2)
            S.barrier()
            if DEBUG and s == 0:
                dma("sp", dbg["h2T"], VW(A_OFF, 32 * K, BF16), reads=["dstT"])
                S.barrier()

            if stop("p3b"):
                break
            o_[0] = D_OFF
            wu = [dalloc(4096, BF16).rearrange("p (k n) -> p k n", k=8) for _ in range(2)]
            ub3 = [dalloc(8224, F32) for _ in range(3)]
            cvb = [dalloc(8192, F32) for _ in range(2)]
            w_up_v = w_up.rearrange("(k p) n -> p k n", p=128)
            for st_ in range(3):
                S.op("pool", lambda e, a=ub3[st_]: e.memset(a[:, 0:8], 0.0), writes=["ub%d" % st_])
            for m in range(22):
                b2 = m % 2
                dma("pool", wu[b2][:, :, 0:128], w_up_v[:, :, 128 * m:128 * m + 128], writes=["wu%d" % b2])
                dma("pool", wu[b2][:, :, 128:256], w_up_v[:, :, DFF + 128 * m:DFF + 128 * m + 128], reads=[], writes=["wu%da" % b2])
                for vg in range(2):
                    ui = (2 * m + vg) % 3
                    un = "ub%d" % ui
                    for tq in range(4):
                        zi = zbank()
                        for k in range(8):
                            mm(PS[zi][:, :], wu[b2][:, k, vg * 128:(vg + 1) * 128], hT[:, k, tq * 512:(tq + 1) * 512], k == 0, k == 7,
                               ["wu%d" % b2, "wu%da" % b2, "hT"], ["ps%d" % zi])
                        cp("act", ub3[ui][:, 8 + tq * 512:8 + (tq + 1) * 512], PS[zi][:, :], ["ps%d" % zi], [un])
                    ch = m + 22 * vg
                    u = ub3[ui]
                    cv = cvb[vg]
                    cn = "cv%d" % vg
                    tsc("pool", cv, u[:, 8:8 + 2048], wconv[:, ch, 2:3], bconv[:, ch:ch + 1], ALU.mult, ALU.add, [un, "wconv", "bconv"], [cn])
                    stt(cv, u[:, 7:7 + 2048], wconv[:, ch, 1:2], cv, ALU.mult, ALU.add, [un, cn, "wconv"], [cn])
                    stt(cv, u[:, 6:6 + 2048], wconv[:, ch, 0:1], cv, ALU.mult, ALU.add, [un, cn, "wconv"], [cn])
                act(cvb[1], cvb[1], AF.Gelu_apprx_tanh, ["cv1"], ["cv1"])
                tt("dve", yT[:, m, :], cvb[1], cvb[0], ALU.mult, ["cv0", "cv1"], ["yT"])
            S.barrier()
            if DEBUG and s == 0:
                dma("sp", dbg["yT"], VW(Y_OFF, 88 * K, BF16), reads=["yT"])
                S.barrier()

            if stop("p4"):
                break
            o_[0] = D_OFF + 4096
            wd = dalloc(44 * K, BF16).rearrange("p (k n) -> p k n", k=22)
            gfin = VW(A_OFF + 16384, 4096, F32)
            xq = [VW(A_OFF + 20480 + i * 4096, 4096, F32) for i in range(2)]
            x2t = [VW(A_OFF + i * 4096, 4096, F32) for i in range(2)]
            ot = [VW(A_OFF + 8192 + i * 4096, 4096, F32) for i in range(2)]
            dma("pool", wd, w_down.rearrange("(k p) n -> p k n", p=128), writes=["wd"])
            dma("sp", gfin, g_fin[0].partition_broadcast(128), writes=["gfin"])
            for t in range(16):
                b2 = t % 2
                tks = slice(t * 128, (t + 1) * 128)
                dma("sp", xq[b2], x1s[s, tks, :], reads=["x1s"], writes=["xq%d" % b2])
                for hh in range(2):
                    pi = 2 * b2 + hh
                    for k in range(22):
                        mm(PS[pi][:, :], yT[:, k, tks], wd[:, k, hh * 512:(hh + 1) * 512], k == 0, k == 21, ["yT", "wd"], ["ps%d" % pi])
                    tt("dve", x2t[b2][:, hh * 512:(hh + 1) * 512], PS[pi][:, :], GT(s, 1)[:, hh * 512:(hh + 1) * 512], ALU.mult,
                       ["ps%d" % pi, "GT"], ["x2t%d" % b2])
                tt("pool", x2t[b2], x2t[b2], xq[b2], ALU.add, ["x2t%d" % b2, "xq%d" % b2], ["x2t%d" % b2])
                rstd = rms_tile(x2t[b2], "x2t%d" % b2, None, None)
                stt(ot[b2], x2t[b2], rstd, gfin, ALU.mult, ALU.mult, ["x2t%d" % b2, "rstd", "gfin"], ["ot%d" % b2])
                dma("sp", out[s, tks, :], ot[b2], reads=["ot%d" % b2], writes=["out"])
            S.barrier()

        S.emit()
    return nc


_NC_CACHE = {}


def kernel(x, c, w_ada, b_ada, g_mix, w_in, w_br_moba, w_br_sb, w_out, rel_bias,
           g_ffn, w_up, w_conv, b_conv, w_down, g_final):
    f = lambda a: np.ascontiguousarray(np.asarray(a, dtype=np.float32))
    x = f(x); c = f(c)
    w_ada = f(w_ada)[0]; b_ada = f(b_ada)[0]; g_mix = f(g_mix)[0]; w_in = f(w_in)[0]
    w_bra = f(w_br_moba)[0]; w_brb = f(w_br_sb)[0]; w_out = f(w_out)[0]; rel_bias = f(rel_bias)
    g_ffn = f(g_ffn)[0]; w_up = f(w_up)[0]; w_conv = f(w_conv)[0]; b_conv = f(b_conv)[0]
    w_down = f(w_down)[0]; g_final = f(g_final)
    k = _consts()
    shared = {
        "w_ada": w_ada,
        "b_adaT": f(b_ada.reshape(48, 128).T),
        "b_ada_row": f(np.concatenate([b_ada[2048:3072], b_ada[5120:6144]])[None, :]),
        "g_mixT": f(g_mix.reshape(8, 128).T),
        "g_ffnT": f(g_ffn.reshape(8, 128).T),
        "g_fin": f(g_final[None, :]),
        "w_in": w_in, "w_bra": w_bra, "w_brb": w_brb, "w_out": w_out, "w_up": w_up,
        "w_convT": f(w_conv.T.reshape(44, 128, 3).transpose(1, 0, 2)),
        "b_convT": f(b_conv.reshape(44, 128).T),
        "w_down": w_down,
        "relG": f(rel_bias[:, k["bidx"]]),
        "b31": f(np.broadcast_to(rel_bias[:, 31][None, :], (128, 8))),
        "k_mmask": k["mmask"], "k_mpast": k["mpast"], "k_negm": k["negm"], "k_ident": k["ident"],
        "k_ustrict": k["ustrict"], "k_sel": k["sel"], "k_inv": k["inv"], "k_selrow": k["selrow"],
    }
    in_maps = []
    for i in range(NCORES):
        m = dict(shared)
        m["x"] = f(x[NSEQ * i:NSEQ * (i + 1)])
        m["cT"] = f(c[NSEQ * i:NSEQ * (i + 1)].reshape(NSEQ, 8, 128).transpose(2, 1, 0))
        in_maps.append(m)
    if "nc" not in _NC_CACHE:
        _NC_CACHE["nc"] = build()
    nc = _NC_CACHE["nc"]
    res = run_bass_kernel_spmd(nc, in_maps, core_ids=list(range(NCORES)))
    kernel.last_results = res
    return np.concatenate([np.asarray(r["out"]) for r in res.results], axis=0).astype(np.float32)
```
